# Optimizing a Trainium2 kernel written in Bass

```python
import jax, jax.numpy as jnp
from jax import lax
import numpy as np

D_MODEL = 1024
BATCH = 2
SEQ = 8192
DEPTH = 1

GRID_W = 64
CTX_LEN = 256
N_HEADS = 6
QK_NOPE_DIM = 128
QK_ROPE_DIM = 64
V_HEAD_DIM = 128
Q_LORA_RANK = 384
KV_LORA_RANK = 256
MLA_WIDTH = N_HEADS * V_HEAD_DIM
FOURIER_GROUPS = 4
FOURIER_GROUP_DIM = 64
FOURIER_WIDTH = FOURIER_GROUPS * FOURIER_GROUP_DIM
MIX_WIDTH = MLA_WIDTH + FOURIER_WIDTH
IN_PROJ_WIDTH = FOURIER_WIDTH + Q_LORA_RANK + KV_LORA_RANK + QK_ROPE_DIM
ROPE_BASE = 10000.0
Q_BLOCK = 128
N_EXPERTS = 32
TOP_K = 4
D_FF = 1024
SWIGLU_LIMIT = 7.0
SWIGLU_ALPHA = 1.702
EXPERT_BLOCK = 128
RMS_EPS = 1e-6
N_MOD = 6

kernel_name = "hybrid_mla_fnet_moe_dit_block"


def rms_norm(x, g):
    xf = x.astype(jnp.float32)
    y = xf * lax.rsqrt(jnp.mean(xf * xf, axis=-1, keepdims=True) + RMS_EPS)
    return (y * g.astype(jnp.float32)).astype(x.dtype)


def modulate(x, g, shift, scale):
    return rms_norm(x, g) * (1 + scale) + shift


def axial_rope_tables(n_tokens):
    rows = n_tokens // GRID_W
    row = jnp.repeat(jnp.arange(rows, dtype=jnp.float32), GRID_W)
    col = jnp.tile(jnp.arange(GRID_W, dtype=jnp.float32), rows)
    half = QK_ROPE_DIM // 2
    inv = 1.0 / (ROPE_BASE ** (jnp.arange(0, half, 2, dtype=jnp.float32) / half))
    ang = jnp.concatenate([row[:, None] * inv, col[:, None] * inv], axis=-1)
    return jnp.cos(ang), jnp.sin(ang)


def rotate(x, cos, sin):
    xf = x.astype(jnp.float32)
    xe, xo = xf[..., 0::2], xf[..., 1::2]
    out = jnp.stack([xe * cos - xo * sin, xe * sin + xo * cos], axis=-1)
    return out.reshape(x.shape).astype(x.dtype)


def mla_project(p, norm_q, norm_kv, w_q_up, w_kv_up, rope):
    B, N, _ = p.shape
    q_lat, kv_lat, k_rope = jnp.split(p, [Q_LORA_RANK, Q_LORA_RANK + KV_LORA_RANK], axis=-1)
    q = (rms_norm(q_lat, norm_q) @ w_q_up).reshape(B, N, N_HEADS, QK_NOPE_DIM + QK_ROPE_DIM)
    kv = (rms_norm(kv_lat, norm_kv) @ w_kv_up).reshape(B, N, N_HEADS, QK_NOPE_DIM + V_HEAD_DIM)
    q_nope, q_rope = jnp.split(q, [QK_NOPE_DIM], axis=-1)
    k_nope, v = jnp.split(kv, [QK_NOPE_DIM], axis=-1)
    if rope is not None:
        cos, sin = rope
        q_rope = rotate(q_rope, cos[:, None, :], sin[:, None, :])
        k_rope = rotate(k_rope, cos, sin)
    k_rope = jnp.broadcast_to(k_rope[:, :, None, :], (B, N, N_HEADS, QK_ROPE_DIM))
    q = jnp.concatenate([q_nope, q_rope], axis=-1)
    k = jnp.concatenate([k_nope, k_rope], axis=-1)
    return q, k, v


def attend(q, k, v):
    B, N, H, Dq = q.shape
    nb = N // Q_BLOCK
    scale = Dq ** -0.5
    qb = jnp.moveaxis(q.reshape(B, nb, Q_BLOCK, H, Dq), 1, 0)

    def one_block(q_blk):
        s = jnp.einsum('bqhd,bkhd->bhqk', q_blk, k, preferred_element_type=jnp.float32) * scale
        pr = jax.nn.softmax(s, axis=-1).astype(v.dtype)
        return jnp.einsum('bhqk,bkhd->bqhd', pr, v)

    o = lax.map(one_block, qb)
    return jnp.moveaxis(o, 0, 1).reshape(B, N, H * V_HEAD_DIM)


def fourier_mix(u, w):
    B, N, _ = u.shape
    ug = u.reshape(B, N, FOURIER_GROUPS, FOURIER_GROUP_DIM).astype(jnp.float32)
    f = jnp.fft.fft2(ug, axes=(1, 3), norm='ortho').real.astype(u.dtype)
    return jnp.einsum('bngc,gcd->bngd', f, w).reshape(B, N, FOURIER_WIDTH)


def moe(h, w_router, b_router, w_gate, b_gate, w_up, b_up, w_down, b_down):
    T, D = h.shape
    logits = jnp.dot(h, w_router, preferred_element_type=jnp.float32) + b_router.astype(jnp.float32)
    top_logit, top_idx = lax.top_k(logits, TOP_K)
    gates = jax.nn.softmax(top_logit, axis=-1)
    n_pairs = T * TOP_K
    flat_e = top_idx.reshape(n_pairs)
    order = jnp.argsort(flat_e)
    sorted_e = flat_e[order]
    pair_tok = order // TOP_K
    pair_gate = gates.reshape(n_pairs)[order].astype(h.dtype)
    counts = jnp.bincount(flat_e, length=N_EXPERTS)
    padded = (counts + EXPERT_BLOCK - 1) // EXPERT_BLOCK * EXPERT_BLOCK
    pad_end = jnp.cumsum(padded)
    pad_start = pad_end - padded
    start = jnp.cumsum(counts) - counts
    dest = pad_start[sorted_e] + jnp.arange(n_pairs) - start[sorted_e]
    n_blocks = -(-n_pairs // EXPERT_BLOCK) + N_EXPERTS
    slot_tok = jnp.zeros((n_blocks * EXPERT_BLOCK,), jnp.int32).at[dest].set(pair_tok.astype(jnp.int32))
    block_e = jnp.minimum(
        jnp.searchsorted(pad_end, jnp.arange(n_blocks) * EXPERT_BLOCK, side='right'), N_EXPERTS - 1)
    xs = h[slot_tok].reshape(n_blocks, EXPERT_BLOCK, D)

    def expert_block(args):
        xb, e = args
        g = jnp.minimum(xb @ w_gate[e] + b_gate[e], SWIGLU_LIMIT)
        u = jnp.clip(xb @ w_up[e] + b_up[e], -SWIGLU_LIMIT, SWIGLU_LIMIT)
        act = g * jax.nn.sigmoid(SWIGLU_ALPHA * g) * (u + 1)
        return act @ w_down[e] + b_down[e]

    ys = lax.map(expert_block, (xs, block_e)).reshape(n_blocks * EXPERT_BLOCK, D)
    y_pairs = ys[dest] * pair_gate[:, None]
    return jnp.zeros_like(h).at[pair_tok].add(y_pairs)


def setup_inputs(seed: int = 0) -> dict:
    key = jax.random.key(seed)
    ks = jax.random.split(key, 32)
    f32 = jnp.float32
    L, D = DEPTH, D_MODEL

    def nrm(k, shape, scale):
        return jax.random.normal(k, shape, f32) * scale

    def gain(k, shape):
        return 1.0 + 0.05 * jax.random.normal(k, shape, f32)

    return {
        'x': nrm(ks[0], (BATCH, SEQ, D), 1.0),
        'c': nrm(ks[1], (BATCH, D), 1.0),
        'ctx': nrm(ks[2], (BATCH, CTX_LEN, D), 1.0),
        'c_ctx': nrm(ks[3], (D,), 1.0),
        'w_mod': nrm(ks[4], (L, D, N_MOD * D), 0.5 * D ** -0.5),
        'b_mod': nrm(ks[5], (L, N_MOD * D), 0.01),
        'norm_attn_pre': gain(ks[6], (L, D)),
        'norm_attn_post': gain(ks[7], (L, D)),
        'norm_ffn_pre': gain(ks[8], (L, D)),
        'norm_ffn_post': gain(ks[9], (L, D)),
        'w_in': nrm(ks[10], (L, D, IN_PROJ_WIDTH), D ** -0.5),
        'norm_q_lat': gain(ks[11], (L, Q_LORA_RANK)),
        'norm_kv_lat': gain(ks[12], (L, KV_LORA_RANK)),
        'w_q_up': nrm(ks[13], (L, Q_LORA_RANK, N_HEADS * (QK_NOPE_DIM + QK_ROPE_DIM)), Q_LORA_RANK ** -0.5),
        'w_kv_up': nrm(ks[14], (L, KV_LORA_RANK, N_HEADS * (QK_NOPE_DIM + V_HEAD_DIM)), KV_LORA_RANK ** -0.5),
        'w_fourier': nrm(ks[15], (L, FOURIER_GROUPS, FOURIER_GROUP_DIM, FOURIER_GROUP_DIM), FOURIER_GROUP_DIM ** -0.5),
        'w_out': nrm(ks[16], (L, MIX_WIDTH, D), MIX_WIDTH ** -0.5),
        'w_router': nrm(ks[17], (L, D, N_EXPERTS), D ** -0.5),
        'b_router': nrm(ks[18], (L, N_EXPERTS), 0.01),
        'w_gate': nrm(ks[19], (L, N_EXPERTS, D, D_FF), D ** -0.5),
        'b_gate': nrm(ks[20], (L, N_EXPERTS, D_FF), 0.01),
        'w_up': nrm(ks[21], (L, N_EXPERTS, D, D_FF), D ** -0.5),
        'b_up': nrm(ks[22], (L, N_EXPERTS, D_FF), 0.01),
        'w_down': nrm(ks[23], (L, N_EXPERTS, D_FF, D), D_FF ** -0.5),
        'b_down': nrm(ks[24], (L, N_EXPERTS, D), 0.01),
    }


def reference(x, c, ctx, c_ctx, w_mod, b_mod, norm_attn_pre, norm_attn_post, norm_ffn_pre,
              norm_ffn_post, w_in, norm_q_lat, norm_kv_lat, w_q_up, w_kv_up, w_fourier, w_out,
              w_router, b_router, w_gate, b_gate, w_up, b_up, w_down, b_down):
    B, N, D = x.shape
    rope = axial_rope_tables(N)
    for l in range(DEPTH):
        last = l == DEPTH - 1
        m = jax.nn.silu(c) @ w_mod[l] + b_mod[l]
        sh1, sc1, g1, sh2, sc2, g2 = jnp.split(m[:, None, :], N_MOD, axis=-1)
        mc = jax.nn.silu(c_ctx) @ w_mod[l] + b_mod[l]
        csh1, csc1, cg1, csh2, csc2, cg2 = jnp.split(mc, N_MOD, axis=-1)

        hx = modulate(x, norm_attn_pre[l], sh1, sc1)
        hc = modulate(ctx, norm_attn_pre[l], csh1, csc1)
        px = hx @ w_in[l]
        pc = hc @ w_in[l]
        qx, kx, vx = mla_project(px[..., FOURIER_WIDTH:], norm_q_lat[l], norm_kv_lat[l],
                                 w_q_up[l], w_kv_up[l], rope)
        qc, kc, vc = mla_project(pc[..., FOURIER_WIDTH:], norm_q_lat[l], norm_kv_lat[l],
                                 w_q_up[l], w_kv_up[l], None)
        k_all = jnp.concatenate([kx, kc], axis=1)
        v_all = jnp.concatenate([vx, vc], axis=1)
        att_x = attend(qx, k_all, v_all)
        four_x = fourier_mix(px[..., :FOURIER_WIDTH], w_fourier[l])
        mix_x = jnp.concatenate([att_x, four_x], axis=-1) @ w_out[l]
        x = x + g1 * rms_norm(mix_x, norm_attn_post[l])
        if not last:
            att_c = attend(qc, kc, vc)
            four_c = fourier_mix(pc[..., :FOURIER_WIDTH], w_fourier[l])
            mix_c = jnp.concatenate([att_c, four_c], axis=-1) @ w_out[l]
            ctx = ctx + cg1 * rms_norm(mix_c, norm_attn_post[l])

        hx2 = modulate(x, norm_ffn_pre[l], sh2, sc2)
        yx = moe(hx2.reshape(B * N, D), w_router[l], b_router[l], w_gate[l], b_gate[l],
                 w_up[l], b_up[l], w_down[l], b_down[l]).reshape(B, N, D)
        x = x + g2 * rms_norm(yx, norm_ffn_post[l])
        if not last:
            hc2 = modulate(ctx, norm_ffn_pre[l], csh2, csc2)
            yc = moe(hc2.reshape(-1, D), w_router[l], b_router[l], w_gate[l], b_gate[l],
                     w_up[l], b_up[l], w_down[l], b_down[l]).reshape(ctx.shape)
            ctx = ctx + cg2 * rms_norm(yc, norm_ffn_post[l])
    return x
```

```python
from contextlib import ExitStack
import numpy as np
import ml_dtypes
import concourse.bass as bass
import concourse.mybir as mybir
from concourse.bass_utils import run_bass_kernel_spmd

F32 = mybir.dt.float32
BF16 = mybir.dt.bfloat16
I32 = mybir.dt.int32
AF = mybir.ActivationFunctionType
OP = mybir.AluOpType
AX = mybir.AxisListType

D = 1024
SEQ = 8192
CTXL = 256
NKT = (SEQ + CTXL) // 128
NKEY = SEQ + CTXL
NOWN = 2048
NTO = NOWN // 128
NH = 6
NE = 32
NSLOT = 96
EPS = 1e-6
QSCALE = 192.0 ** -0.5


class Sch:
    def __init__(self, nc, es):
        self.nc = nc
        self.es = es
        self.eng = {"pe": nc.tensor, "act": nc.scalar, "dve": nc.vector, "pool": nc.gpsimd, "sp": nc.sync}
        self.sem = {}
        self.cnt = {}
        for k in self.eng:
            self.sem[k] = es.enter_context(nc.semaphore("sem_" + k))
            self.cnt[k] = 0
        self.waited = {k: {} for k in self.eng}
        self.last_w = {}
        self.readers = {}
        self.n2p = {}

    def dsem(self, name, sw=False):
        name = ("G:" if sw else "H:") + name
        if name not in self.n2p:
            pk = ("DG%d" if sw else "DH%d") % sum(1 for k in self.n2p if k.startswith(name[:2]))
            if pk not in self.sem:
                self.sem[pk] = self.es.enter_context(self.nc.semaphore("dsem_" + pk))
                self.cnt[pk] = 0
            self.n2p[name] = pk
        return self.n2p[name]

    def _wait(self, e, s, v):
        if v <= 0:
            return
        if s == e and e == "pe":
            return
        if s.startswith("D"):
            v = max(v, self.cnt[s])
        if self.waited[e].get(s, 0) >= v:
            return
        self.eng[e].wait_ge(self.sem[s], v)
        self.waited[e][s] = v

    def _deps(self, e, r, w):
        for k in r:
            ev = self.last_w.get(k)
            if ev:
                self._wait(e, *ev)
        for k in w:
            ev = self.last_w.get(k)
            if ev:
                self._wait(e, *ev)
            for ev in self.readers.get(k, ()):
                self._wait(e, *ev)

    def _record(self, me, r, w):
        for k in r:
            self.readers.setdefault(k, []).append(me)
        for k in w:
            self.last_w[k] = me
            self.readers[k] = []

    def op(self, e, fn, r=(), w=()):
        self._deps(e, r, w)
        ins = fn(self.eng[e])
        self.cnt[e] += 1
        ins.then_inc(self.sem[e], 1)
        self._record((e, self.cnt[e]), r, w)
        return ins

    def dma(self, q, sname, out, in_, r=(), w=(), **kw):
        sname = self.dsem(sname, sw=(q == "pool"))
        self._deps(q, r, w)
        self._wait(q, sname, self.cnt[sname])
        ins = self.eng[q].dma_start(out=out, in_=in_, **kw)
        self.cnt[sname] += 16
        ins.then_inc(self.sem[sname], 16)
        self._record((sname, self.cnt[sname]), r, w)
        return ins

    def idma(self, sname, r=(), w=(), **kw):
        sname = self.dsem(sname, sw=True)
        self._deps("pool", r, w)
        self._wait("pool", sname, self.cnt[sname])
        ins = self.nc.gpsimd.indirect_dma_start(**kw)
        self.cnt[sname] += 16
        ins.then_inc(self.sem[sname], 16)
        self._record((sname, self.cnt[sname]), r, w)
        return ins

    def barrier(self):
        for e in self.eng:
            for s in list(self.sem):
                if s != e:
                    self._wait(e, s, self.cnt[s])
        self.last_w = {}
        self.readers = {}
        self.n2p = {}


def build_nc(stage=99, dbg=()):
    nc = bass.Bass("TRN2", target_bir_lowering=False)
    es = ExitStack()

    def dram(name, shape, dt, kind="ExternalInput"):
        return nc.dram_tensor(name, list(shape), dt, kind=kind).ap()

    x_b = dram("x_b", [SEQ, D], F32)
    x_own = dram("x_own", [NOWN, D], F32)
    ctx_b = dram("ctx_b", [CTXL, D], F32)
    c_col = dram("c_col", [128, 8], F32)
    cc_col = dram("cc_col", [128, 8], F32)
    w_mod = dram("w_mod", [D, 6 * D], F32)
    b_mod = dram("b_mod", [1, 6 * D], F32)
    g_attn_pre = dram("g_attn_pre", [1, D], F32)
    g_attn_post = dram("g_attn_post", [1, D], F32)
    g_ffn_pre = dram("g_ffn_pre", [1, D], F32)
    g_ffn_post = dram("g_ffn_post", [1, D], F32)
    w_in = dram("w_in", [D, 960], F32)
    g_q = dram("g_q", [1, 384], F32)
    g_kv = dram("g_kv", [1, 256], F32)
    w_q_up = dram("w_q_up", [384, 1152], F32)
    w_kv_up = dram("w_kv_up", [256, 1536], F32)
    w_four = dram("w_four", [256, 64], F32)
    w_out = dram("w_out", [D, D], F32)
    w_router = dram("w_router", [D, NE], F32)
    b_router = dram("b_router", [1, NE], F32)
    wa_gate = dram("wa_gate", [NE * 128, 9 * D], F32)
    wa_up = dram("wa_up", [NE * 128, 9 * D], F32)
    wa_down = dram("wa_down", [NE * 128, 9 * D], F32)
    wb_gate = dram("wb_gate", [NE * 128, 9 * D], BF16, kind="Internal")
    wb_up = dram("wb_up", [NE * 128, 9 * D], BF16, kind="Internal")
    wb_down = dram("wb_down", [NE * 128, 9 * D], BF16, kind="Internal")
    rope_all = dram("rope_all", [SEQ, 128], F32)
    rope_own = dram("rope_own", [NOWN, 128], F32)
    dft_c = dram("dft_c", [SEQ, NOWN], BF16)
    dft_s = dram("dft_s", [SEQ, NOWN], BF16)
    cbd = dram("cbd", [256, 512], F32)
    ident_d = dram("ident_d", [128, 128], F32)
    tri_d = dram("tri_d", [128, 128], F32)
    iota_d = dram("iota_d", [128, NE + NSLOT + 1], F32)
    uu_d = dram("uu_d", [NE, 2 * NE], F32)
    out_d = dram("out", [NOWN, D], F32, kind="ExternalOutput")
    xs_d = dram("xs_scr", [NSLOT * 128, D + 32], BF16, kind="Internal")
    gs_d = dram("gs_scr", [NSLOT * 128, 16], F32, kind="Internal")
    ys_d = dram("ys_scr", [NSLOT * 128, D], F32, kind="Internal")
    dbg_out = {}

    S = Sch(nc, es)

    def sb(name, shape, dt):
        return es.enter_context(nc.sbuf_tensor(name, list(shape), dt))

    def add_dbg(name, tile_ap, shape, dt=F32):
        if name in dbg:
            d_ = dram("dbg_" + name, shape, dt, kind="ExternalOutput")
            dbg_out[name] = (d_, tile_ap)

    ident_f = sb("ident_f", [128, 128], F32)
    ident_b = sb("ident_b", [128, 128], BF16)
    ones_b = sb("ones_b", [128, 128], BF16)
    ones_f = sb("ones_f", [128, 128], F32)
    S.dma("sp", "c0", ident_f[:], ident_d, w=["ident_f"])
    S.op("dve", lambda e: e.tensor_copy(out=ident_b[:], in_=ident_f[:]), r=["ident_f"], w=["ident_b"])
    S.op("dve", lambda e: e.memset(ones_b[:], 1.0), w=["ones_b"])
    S.op("dve", lambda e: e.memset(ones_f[:], 1.0), w=["ones_f"])

    epsb = sb("epsb", [128, 1], F32)
    S.op("dve", lambda e: e.memset(epsb[:], EPS), w=["epsb"])

    modn = ["A1", "B1", "cA1", "cB1", "A2", "B2", "G1", "G2"]
    mod = {n: sb("mod_" + n, [128, D], F32) for n in modn}

    def bcast_row(name, src_row, n, q="sp"):
        t = sb(name, [128, n], F32)
        S.dma(q, "c0", t[:], src_row.partition_broadcast(128), w=[name])
        return t

    with ExitStack() as p0:
        def sb0(name, shape, dt):
            return p0.enter_context(nc.sbuf_tensor(name, list(shape), dt))

        def ps0(name, shape, dt):
            return p0.enter_context(nc.psum_tensor(name, list(shape), dt))
        ccol = sb0("ccol", [128, 16], F32)
        S.dma("sp", "c0", ccol[:, 0:8], c_col, w=["ccol"])
        S.dma("sp", "c0", ccol[:, 8:16], cc_col, w=["ccol"])
        scol = sb0("scol", [128, 16], F32)
        S.op("act", lambda e: e.activation(out=scol[:], in_=ccol[:], func=AF.Silu), r=["ccol"], w=["scol"])
        sbc = sb0("sbc", [128, 16, 128], F32)
        for kc in range(16):
            S.op("dve", lambda e, kc=kc: e.tensor_scalar(out=sbc[:, kc, :], in0=ones_f[:], scalar1=scol[:, kc:kc + 1],
                                                      scalar2=None, op0=OP.mult), r=["scol", "ones_f"], w=["sbc"])
        gap = sb0("gap", [128, D], F32)
        gpo = sb0("gpo", [128, D], F32)
        gfp = sb0("gfp", [128, D], F32)
        gfo = sb0("gfo", [128, D], F32)
        for t, src, nm in ((gap, g_attn_pre, "gap"), (gpo, g_attn_post, "gpo"), (gfp, g_ffn_pre, "gfp"), (gfo, g_ffn_post, "gfo")):
            S.dma("sp", "c0", t[:], src.partition_broadcast(128), w=[nm])
        wm = [sb0("wm%d" % i, [128, 8, 512], F32) for i in range(2)]
        bm = [sb0("bm%d" % i, [128, 512], F32) for i in range(2)]
        pm = [ps0("pm%d" % i, [128, 512], F32) for i in range(2)]
        jobs = [(nb, 0) for nb in range(12)] + [(nb, 1) for nb in range(4)]
        for it, (nb, isctx) in enumerate(jobs):
            bi = it % 2
            wk, bk, pk = "wm%d" % bi, "bm%d" % bi, "pm%d" % bi
            S.dma("sp", wk, wm[bi][:], w_mod[:, nb * 512:(nb + 1) * 512].rearrange("(kc p) n -> p kc n", p=128), w=[wk])
            S.dma("sp", wk, bm[bi][:], b_mod[:, nb * 512:(nb + 1) * 512].partition_broadcast(128), w=[bk])
            for kc in range(8):
                S.op("pe", lambda e, kc=kc, bi=bi, isctx=isctx: e.matmul(pm[bi][:], lhsT=sbc[:, isctx * 8 + kc, :], rhs=wm[bi][:, kc, :],
                                                                   start=(kc == 0), stop=(kc == 7)),
                     r=[wk, "sbc"], w=[pk])
            half = slice((nb % 2) * 512, (nb % 2) * 512 + 512)
            grp = nb // 2
            if isctx:
                dst = {0: "cB1", 1: "cA1"}[grp]
            else:
                dst = {0: "B1", 1: "A1", 2: "G1", 3: "B2", 4: "A2", 5: "G2"}[grp]
            gsrc = {"A1": gap, "cA1": gap, "A2": gfp, "G1": gpo, "G2": gfo}.get(dst)
            gnm = {"A1": "gap", "cA1": "gap", "A2": "gfp", "G1": "gpo", "G2": "gfo"}.get(dst)
            dt_ = mod[dst]
            S.op("dve", lambda e, dt_=dt_, bi=bi, half=half: e.tensor_tensor(out=dt_[:, half], in0=pm[bi][:], in1=bm[bi][:], op=OP.add),
                 r=[pk, bk], w=[dst])
            if dst in ("A1", "cA1", "A2"):
                S.op("dve", lambda e, dt_=dt_, half=half, gsrc=gsrc: e.scalar_tensor_tensor(out=dt_[:, half], in0=dt_[:, half], scalar=1.0,
                                                                                        in1=gsrc[:, half], op0=OP.add, op1=OP.mult),
                     r=[dst, gnm], w=[dst])
            elif dst in ("G1", "G2"):
                S.op("dve", lambda e, dt_=dt_, half=half, gsrc=gsrc: e.tensor_tensor(out=dt_[:, half], in0=dt_[:, half], in1=gsrc[:, half], op=OP.mult),
                     r=[dst, gnm], w=[dst])
        S.barrier()
    for n in modn:
        add_dbg(n, mod[n][:], [128, D])


    def flush_dbg():
        for name, (d_, ap_) in list(dbg_out.items()):
            if ap_ is not None:
                S.dma("sp", "dbg", d_, ap_, r=[], w=[])
                dbg_out[name] = (d_, None)
        S.barrier()

    flush_dbg()
    if stage < 1:
        S.barrier(); es.close(); return nc, list(dbg_out)

    x1_d = dram("x1_scr", [NOWN, D], F32, kind="Internal")
    h2_d = dram("h2_scr", [NOWN, D], BF16, kind="Internal")

    scA = ExitStack()
    sc2 = ExitStack()
    sc2q = ExitStack()
    sc3 = ExitStack()

    def close_all():
        sc2q.close(); sc2.close(); sc3.close(); scA.close()

    def sbA(name, shape, dt):
        return scA.enter_context(nc.sbuf_tensor(name, list(shape), dt))
    kvnT = sbA("kvnT", [128, 2, NKEY], BF16)
    kropeT = sbA("kropeT", [128, NKEY], BF16)
    fourT = sbA("fourT", [128, 2, NOWN], BF16)

    def rstd(stt, ci, co, key):
        S.op("act", lambda e: e.activation(out=stt[:, co:co + 1], in_=stt[:, ci:ci + 1], func=AF.Sqrt, bias=epsb[:, 0:1], scale=1.0), r=[key, "epsb"], w=[key])
        S.op("dve", lambda e: e.reciprocal(out=stt[:, co:co + 1], in_=stt[:, co:co + 1]), r=[key], w=[key])

    def hx_pipeline(sbx, ti, src_ap, Akey, Bkey, xin, st, tmpf, hxb, tp_hx, hxT, junk):
        i3, i2 = ti % 3, ti % 2
        S.op("act", lambda e: e.activation(out=junk[:], in_=xin[i3][:], func=AF.Square, scale=1.0 / 32.0, accum_out=st[i2][:, 0:1]),
             r=["xin%d" % i3], w=["junk", "st%d" % i2])
        rstd(st[i2], 0, 1, "st%d" % i2)
        S.op("dve", lambda e: e.scalar_tensor_tensor(out=tmpf[i2][:], in0=xin[i3][:], scalar=st[i2][:, 1:2], in1=mod[Akey][:],
                                                     op0=OP.mult, op1=OP.mult),
             r=["xin%d" % i3, "st%d" % i2], w=["tmpf%d" % i2])
        S.op("pool", lambda e: e.tensor_tensor(out=hxb[i2][:], in0=tmpf[i2][:], in1=mod[Bkey][:], op=OP.add),
             r=["tmpf%d" % i2], w=["hxb%d" % i2])
        for kc in range(8):
            S.op("pe", lambda e, kc=kc: e.transpose(out=tp_hx[i2][:, kc, :], in_=hxb[i2][:, kc * 128:(kc + 1) * 128], identity=ident_b[:]),
                 r=["hxb%d" % i2], w=["tp_hx%d" % i2])
        S.op("act", lambda e: e.activation(out=hxT[i2][:], in_=tp_hx[i2][:], func=AF.Copy), r=["tp_hx%d" % i2], w=["hxT%d" % i2])

    sc1 = ExitStack()

    def sb1(name, shape, dt):
        return sc1.enter_context(nc.sbuf_tensor(name, list(shape), dt))

    def ps1(name, shape, dt):
        return sc1.enter_context(nc.psum_tensor(name, list(shape), dt))
    PQ = sb1("PQ", [128, 64, 512], BF16)
    w1 = sb1("w1", [128, 8, 832], BF16)
    gkv = sb1("gkv", [128, 256], F32)
    S.dma("sp", "c0", gkv[:], g_kv.partition_broadcast(128), w=["gkv"])
    S.dma("pool", "w1a", w1[:, :, 0:320], w_in[:, 640:960].rearrange("(kc p) n -> p kc n", p=128), w=["w1"])
    with ExitStack() as pw:
        def sbw(name, shape, dt):
            return pw.enter_context(nc.sbuf_tensor(name, list(shape), dt))

        def psw(name, shape, dt):
            return pw.enter_context(nc.psum_tensor(name, list(shape), dt))
        cbd_sb = sbw("cbd_sb", [128, 2, 512], F32)
        wf_sb = sbw("wf_sb", [128, 2, 64], F32)
        mbd = sbw("mbd", [128, 2, 512], BF16)
        wu = sbw("wu", [128, 8, 256], BF16)
        wuT = sbw("wuT", [128, 2, D], BF16)
        S.dma("sp", "c0", cbd_sb[:], cbd.rearrange("(kc p) n -> p kc n", p=128), w=["cbd_sb"])
        S.dma("sp", "c0", wf_sb[:], w_four.rearrange("(kc p) n -> p kc n", p=128), w=["wf_sb"])
        S.dma("pool", "w1a", wu[:], w_in[:, 0:256].rearrange("(kc p) n -> p kc n", p=128), w=["wu"])
        S.op("dve", lambda e: e.memset(mbd[:], 0.0), w=["mbd"])
        pcw = psw("pcw", [128, 512], F32)
        for mc in range(2):
            for tr in range(2):
                o = (mc * 2 + tr) * 64
                S.op("pe", lambda e, mc=mc, tr=tr, o=o: e.matmul(pcw[:, o:o + 64], lhsT=cbd_sb[:, mc, tr * 256 + mc * 128: tr * 256 + mc * 128 + 128],
                                                              rhs=wf_sb[:, mc, :], start=True, stop=True),
                     r=["cbd_sb", "wf_sb"], w=["pcw"])
        for mc in range(2):
            for tr in range(2):
                o = (mc * 2 + tr) * 64
                for gl in range(2):
                    g = 2 * mc + gl
                    S.op("dve", lambda e, mc=mc, tr=tr, o=o, gl=gl, g=g: e.tensor_copy(
                        out=mbd[gl * 64:(gl + 1) * 64, mc, tr * 256 + g * 64: tr * 256 + (g + 1) * 64],
                        in_=pcw[gl * 64:(gl + 1) * 64, o:o + 64]), r=["pcw"], w=["mbd"])
        ptw = [psw("ptw%d" % i, [128, 1024], BF16) for i in range(2)]
        for kc in range(2):
            for dc in range(8):
                S.op("pe", lambda e, kc=kc, dc=dc: e.transpose(out=ptw[kc][:, dc * 128:(dc + 1) * 128], in_=wu[:, dc, kc * 128:(kc + 1) * 128],
                                                            identity=ident_b[:]), r=["wu", "ident_b"], w=["ptw%d" % kc])
            S.op("act", lambda e, kc=kc: e.activation(out=wuT[:, kc, :], in_=ptw[kc][:], func=AF.Copy), r=["ptw%d" % kc], w=["wuT"])
        ppq = [psw("ppq%d" % i, [128, 512], F32) for i in range(2)]
        for dc in range(8):
            for kc in range(2):
                S.op("pe", lambda e, kc=kc, dc=dc: e.matmul(ppq[dc % 2][:], lhsT=wuT[:, kc, dc * 128:(dc + 1) * 128], rhs=mbd[:, kc, :],
                                                         start=(kc == 0), stop=(kc == 1)), r=["wuT", "mbd"], w=["ppq%d" % (dc % 2)])
            S.op("dve", lambda e, dc=dc: e.tensor_copy(out=w1[:, dc, 320:832], in_=ppq[dc % 2][:]), r=["ppq%d" % (dc % 2)], w=["w1"])
        S.barrier()

    sc1t = ExitStack()

    def sb1(name, shape, dt):
        return sc1t.enter_context(nc.sbuf_tensor(name, list(shape), dt))

    def ps1(name, shape, dt):
        return sc1t.enter_context(nc.psum_tensor(name, list(shape), dt))
    xin = [sb1("xin%d" % i, [128, D], F32) for i in range(3)]
    st = [sb1("st%d" % i, [128, 8], F32) for i in range(2)]
    tmpf = [sb1("tmpf%d" % i, [128, D], F32) for i in range(2)]
    hxb = [sb1("hxb%d" % i, [128, D], BF16) for i in range(2)]
    hxT = [sb1("hxT%d" % i, [128, 8, 128], BF16) for i in range(2)]
    junk = sb1("junk", [128, D], F32)
    rp = [sb1("rp%d" % i, [128, 128], F32) for i in range(2)]
    kraw = [sb1("kraw%d" % i, [128, 64], F32) for i in range(2)]
    rt = [sb1("rt%d" % i, [128, 128], F32) for i in range(2)]
    kvr = [sb1("kvr%d" % i, [128, 384], BF16) for i in range(2)]
    tp_hx = [ps1("tp_hx%d" % i, [128, 8, 128], BF16) for i in range(2)]
    ps_kv = [ps1("ps_kv%d" % i, [128, 512], F32) for i in range(2)]
    ps_pq = [ps1("ps_pq%d" % i, [128, 512], F32) for i in range(2)]
    tp_kv = ps1("tp_kv", [128, 8, 128], BF16)

    n_t1 = NKT if stage >= 2 else 4
    for ti in range(n_t1):
        isctx = ti >= 64
        i2 = ti % 2
        def stage_a(t_):
            c_ = t_ >= 64
            src_ = ctx_b[(t_ - 64) * 128:(t_ - 63) * 128, :] if c_ else x_b[t_ * 128:(t_ + 1) * 128, :]
            hx_pipeline(sb1, t_, src_, "cA1" if c_ else "A1", "cB1" if c_ else "B1", xin, st, tmpf, hxb, tp_hx, hxT, junk)
        def load_x(t_):
            c_ = t_ >= 64
            src_ = ctx_b[(t_ - 64) * 128:(t_ - 63) * 128, :] if c_ else x_b[t_ * 128:(t_ + 1) * 128, :]
            S.dma("sp", "xin%d" % (t_ % 3), xin[t_ % 3][:], src_, w=["xin%d" % (t_ % 3)])
        if ti == 0:
            load_x(0)
            if n_t1 > 1:
                load_x(1)
            stage_a(0)
        if ti + 2 < n_t1:
            load_x(ti + 2)
        if ti + 1 < n_t1:
            stage_a(ti + 1)
        if ti == 0:
            add_dbg("hxb0", hxb[0][:], [128, D], BF16)
            flush_dbg()
        if not isctx:
            S.dma("sp", "rp%d" % i2, rp[i2][:], rope_all[ti * 128:(ti + 1) * 128, :], w=["rp%d" % i2])
        for kc in range(8):
            S.op("pe", lambda e, kc=kc: e.matmul(ps_kv[i2][:, 0:320], lhsT=hxT[i2][:, kc, :], rhs=w1[:, kc, 0:320], start=(kc == 0), stop=(kc == 7)),
                 r=["hxT%d" % i2, "w1"], w=["ps_kv%d" % i2])
        if not isctx:
            for kc in range(8):
                S.op("pe", lambda e, kc=kc: e.matmul(ps_pq[i2][:], lhsT=hxT[i2][:, kc, :], rhs=w1[:, kc, 320:832], start=(kc == 0), stop=(kc == 7)),
                     r=["hxT%d" % i2, "w1"], w=["ps_pq%d" % i2])
        S.op("act", lambda e: e.activation(out=junk[:, 0:256], in_=ps_kv[i2][:, 0:256], func=AF.Square, scale=1.0 / 16.0, accum_out=st[i2][:, 2:3]),
             r=["ps_kv%d" % i2], w=["junk", "st%d" % i2])
        rstd(st[i2], 2, 3, "st%d" % i2)
        S.op("dve", lambda e: e.scalar_tensor_tensor(out=kvr[i2][:, 0:256], in0=ps_kv[i2][:, 0:256], scalar=st[i2][:, 3:4], in1=gkv[:],
                                                     op0=OP.mult, op1=OP.mult), r=["ps_kv%d" % i2, "st%d" % i2, "gkv"], w=["kvr%d" % i2])
        if isctx:
            for o_ in (256, 320):
                S.op("act", lambda e, o_=o_: e.activation(out=kvr[i2][:, o_:o_ + 64], in_=ps_kv[i2][:, 256:320], func=AF.Copy),
                     r=["ps_kv%d" % i2], w=["kvr%d" % i2])
        else:
            S.op("act", lambda e: e.activation(out=kraw[i2][:], in_=ps_kv[i2][:, 256:320], func=AF.Copy), r=["ps_kv%d" % i2], w=["kraw%d" % i2])
            S.op("pool", lambda e: e.tensor_tensor(out=rt[i2][:, 0:64], in0=kraw[i2][:], in1=rp[i2][:, 0:64], op=OP.mult),
                 r=["kraw%d" % i2, "rp%d" % i2], w=["rt%d" % i2])
            S.op("pool", lambda e: e.tensor_tensor(out=rt[i2][:, 64:128:2], in0=kraw[i2][:, 1:64:2], in1=rp[i2][:, 64:128:2], op=OP.mult),
                 r=["kraw%d" % i2, "rp%d" % i2], w=["rt%d" % i2])
            S.op("pool", lambda e: e.tensor_tensor(out=rt[i2][:, 65:128:2], in0=kraw[i2][:, 0:64:2], in1=rp[i2][:, 65:128:2], op=OP.mult),
                 r=["kraw%d" % i2, "rp%d" % i2], w=["rt%d" % i2])
            for o_ in (256, 320):
                S.op("pool", lambda e, o_=o_: e.tensor_tensor(out=kvr[i2][:, o_:o_ + 64], in0=rt[i2][:, 0:64], in1=rt[i2][:, 64:128], op=OP.add),
                     r=["rt%d" % i2], w=["kvr%d" % i2])
            S.op("act", lambda e: e.activation(out=PQ[:, ti, :], in_=ps_pq[i2][:], func=AF.Copy), r=["ps_pq%d" % i2], w=[("PQ", ti)])
        for c3 in range(3):
            S.op("pe", lambda e, c3=c3: e.transpose(out=tp_kv[:, c3, :], in_=kvr[i2][:, c3 * 128:c3 * 128 + 128], identity=ident_b[:]),
                 r=["kvr%d" % i2], w=["tp_kv"])
        S.op("dve", lambda e: e.tensor_copy(out=kvnT[:, :, ti * 128:(ti + 1) * 128], in_=tp_kv[:, 0:2, :]), r=["tp_kv"], w=[("kvnT", ti)])
        S.op("dve", lambda e: e.tensor_copy(out=kropeT[:, ti * 128:(ti + 1) * 128], in_=tp_kv[:, 2, :]), r=["tp_kv"], w=[("kropeT", ti)])
    S.barrier()
    add_dbg("kvnT", kvnT[:, :, 0:512], [128, 2, 512], BF16)
    add_dbg("kropeT", kropeT[:, 0:512], [128, 512], BF16)
    add_dbg("PQ0", PQ[:, 0, :], [128, 512], BF16)
    flush_dbg()
    sc1t.close()
    if stage < 3:
        sc1.close(); close_all(); S.barrier(); es.close(); return nc, list(dbg_out)

    with ExitStack() as pf_:
        tabc = [pf_.enter_context(nc.sbuf_tensor("tabc%d" % i, [128, NOWN], BF16)) for i in range(4)]
        tabs = [pf_.enter_context(nc.sbuf_tensor("tabs%d" % i, [128, NOWN], BF16)) for i in range(4)]
        pf = [[pf_.enter_context(nc.psum_tensor("pf%d_%d" % (c, kb), [128, 512], F32)) for kb in range(4)] for c in range(2)]
        for t in range(64):
            i2 = t % 4
            S.dma("sp", "tabc%d" % i2, tabc[i2][:], dft_c[t * 128:(t + 1) * 128, :], w=["tabc%d" % i2])
            S.dma("pool", "tabs%d" % i2, tabs[i2][:], dft_s[t * 128:(t + 1) * 128, :], w=["tabs%d" % i2])
            for c in range(2):
                for (tab, tn, off) in ((tabc, "tabc", 0), (tabs, "tabs", 256)):
                    for kb in range(4):
                        S.op("pe", lambda e, c=c, tab=tab, off=off, kb=kb: e.matmul(
                            pf[c][kb][:], lhsT=PQ[:, t, off + c * 128: off + c * 128 + 128], rhs=tab[i2][:, kb * 512:(kb + 1) * 512],
                            start=(t == 0 and off == 0), stop=(t == 63 and off == 256)),
                            r=["%s%d" % (tn, i2)], w=["pf%d_%d" % (c, kb)])
        for c in range(2):
            for kb in range(4):
                eng = "act" if kb % 2 == 0 else "dve"
                if eng == "act":
                    S.op("act", lambda e, c=c, kb=kb: e.activation(out=fourT[:, c, kb * 512:(kb + 1) * 512], in_=pf[c][kb][:], func=AF.Copy),
                         r=["pf%d_%d" % (c, kb)], w=["fourT"])
                else:
                    S.op("dve", lambda e, c=c, kb=kb: e.tensor_copy(out=fourT[:, c, kb * 512:(kb + 1) * 512], in_=pf[c][kb][:]),
                         r=["pf%d_%d" % (c, kb)], w=["fourT"])
        S.barrier()
    add_dbg("fourT", fourT[:], [128, 2, NOWN], BF16)
    flush_dbg()
    sc1.close()
    if stage < 4:
        close_all(); S.barrier(); es.close(); return nc, list(dbg_out)


    def sb2q(name, shape, dt):
        return sc2q.enter_context(nc.sbuf_tensor(name, list(shape), dt))
    q_nopeT = sb2q("q_nopeT", [128, NH, NOWN], BF16)
    q_ropeT = sb2q("q_ropeT", [128, 3, NOWN], BF16)
    wkv = sb2q("wkv", [128, 2, 1536], BF16)
    S.dma("pool", "wl", wkv[:], w_kv_up.rearrange("(kc p) n -> p kc n", p=128), w=["wkv"])
    sc2t = ExitStack()

    def sb2(name, shape, dt):
        return sc2t.enter_context(nc.sbuf_tensor("b_" + name, list(shape), dt, side="right"))

    def ps2(name, shape, dt):
        return sc2t.enter_context(nc.psum_tensor("b_" + name, list(shape), dt))
    w2 = sb2("w2", [128, 8, 384], BF16)
    wq = sb2("wq", [128, 3, 1152], BF16)
    gq = sb2("gq", [128, 384], F32)
    S.dma("pool", "wl", w2[:], w_in[:, 256:640].rearrange("(kc p) n -> p kc n", p=128), w=["w2"])
    S.dma("pool", "wl", wq[:], w_q_up.rearrange("(kc p) n -> p kc n", p=128), w=["wq"])
    S.dma("sp", "c0", gq[:], g_q.partition_broadcast(128), w=["gq"])
    xin = [sb2("xin%d" % i, [128, D], F32) for i in range(3)]
    st = [sb2("st%d" % i, [128, 8], F32) for i in range(2)]
    tmpf = [sb2("tmpf%d" % i, [128, D], F32) for i in range(2)]
    hxb = [sb2("hxb%d" % i, [128, D], BF16) for i in range(2)]
    hxT = [sb2("hxT%d" % i, [128, 8, 128], BF16) for i in range(2)]
    junk = sb2("junk", [128, D], F32)
    rp = [sb2("rp%d" % i, [128, 128], F32) for i in range(2)]
    qnb = [sb2("qnb%d" % i, [128, 384], BF16) for i in range(2)]
    qnT = [sb2("qnT%d" % i, [128, 3, 128], BF16) for i in range(2)]
    qf = [sb2("qf%d" % i, [128, NH, 192], F32) for i in range(2)]
    qbn = [sb2("qbn%d" % i, [128, NH, 128], BF16) for i in range(2)]
    rq = [sb2("rq%d" % i, [128, NH, 128], F32) for i in range(2)]
    qrp = [sb2("qrp%d" % i, [128, NH, 64], BF16) for i in range(2)]
    tp_hx = [ps2("tp_hx%d" % i, [128, 8, 128], BF16) for i in range(2)]
    ps_q1 = ps2("ps_q1", [128, 512], F32)
    tp_qr = ps2("tp_qr", [128, 8, 128], BF16)
    tp_q = tp_qr[:, 0:3, :]
    ps_q = [ps2("ps_qq%d" % i, [128, 512], F32) for i in range(3)]
    tpn = ps2("tpn", [128, 8, 128], BF16)
    tpr = tp_qr[:, 3:6, :]

    def hx_keys_fix(i2):
        return "tp_hx0"
    n_t1b = NTO if stage >= 5 else 2
    for ti in range(n_t1b):
        i2 = ti % 2
        def load_xo(t_):
            S.dma("sp", "xin%d" % (t_ % 3), xin[t_ % 3][:], x_own[t_ * 128:(t_ + 1) * 128, :], w=["xin%d" % (t_ % 3)])
        if ti == 0:
            load_xo(0)
            if n_t1b > 1:
                load_xo(1)
            hx_pipeline(sb2, 0, x_own[0:128, :], "A1", "B1", xin, st, tmpf, hxb, tp_hx, hxT, junk)
        if ti + 2 < n_t1b:
            load_xo(ti + 2)
        if ti + 1 < n_t1b:
            hx_pipeline(sb2, ti + 1, x_own[(ti + 1) * 128:(ti + 2) * 128, :], "A1", "B1", xin, st, tmpf, hxb, tp_hx, hxT, junk)
        S.dma("sp", "rp%d" % i2, rp[i2][:], rope_own[ti * 128:(ti + 1) * 128, :], w=["rp%d" % i2])
        for kc in range(8):
            S.op("pe", lambda e, kc=kc: e.matmul(ps_q1[:, 0:384], lhsT=hxT[i2][:, kc, :], rhs=w2[:, kc, :], start=(kc == 0), stop=(kc == 7)),
                 r=["hxT%d" % i2, "w2"], w=["ps_q1"])
        S.op("act", lambda e: e.activation(out=junk[:, 0:384], in_=ps_q1[:, 0:384], func=AF.Square, scale=float(384.0 ** -0.5), accum_out=st[i2][:, 2:3]),
             r=["ps_q1"], w=["junk", "st%d" % i2])
        rstd(st[i2], 2, 3, "st%d" % i2)
        S.op("dve", lambda e: e.scalar_tensor_tensor(out=qnb[i2][:], in0=ps_q1[:, 0:384], scalar=st[i2][:, 3:4], in1=gq[:], op0=OP.mult, op1=OP.mult),
             r=["ps_q1", "st%d" % i2, "gq"], w=["qnb%d" % i2])
        for c3 in range(3):
            S.op("pe", lambda e, c3=c3: e.transpose(out=tp_q[:, c3, :], in_=qnb[i2][:, c3 * 128:(c3 + 1) * 128], identity=ident_b[:]),
                 r=["qnb%d" % i2], w=["tp_q"])
        S.op("act", lambda e: e.activation(out=qnT[i2][:], in_=tp_q, func=AF.Copy), r=["tp_q"], w=["qnT%d" % i2])
        qf_flat = qf[i2][:].rearrange("p h d -> p (h d)")
        for nb, (c0, c1) in enumerate(((0, 512), (512, 1024), (1024, 1152))):
            for kc in range(3):
                S.op("pe", lambda e, kc=kc, nb=nb, c0=c0, c1=c1: e.matmul(ps_q[nb][:, 0:c1 - c0], lhsT=qnT[i2][:, kc, :], rhs=wq[:, kc, c0:c1],
                                                                      start=(kc == 0), stop=(kc == 2)), r=["qnT%d" % i2, "wq"], w=["ps_q%d" % nb])
            S.op("act", lambda e, nb=nb, c0=c0, c1=c1: e.activation(out=qf_flat[:, c0:c1], in_=ps_q[nb][:, 0:c1 - c0], func=AF.Copy),
                 r=["ps_q%d" % nb], w=["qf%d" % i2])
        S.op("dve", lambda e: e.tensor_copy(out=qbn[i2][:], in_=qf[i2][:, :, 0:128]), r=["qf%d" % i2], w=["qbn%d" % i2])
        cosb = rp[i2][:, 0:64].unsqueeze(1).to_broadcast([128, NH, 64])
        sineb = rp[i2][:, 64:128:2].unsqueeze(1).to_broadcast([128, NH, 32])
        sinob = rp[i2][:, 65:128:2].unsqueeze(1).to_broadcast([128, NH, 32])
        S.op("pool", lambda e: e.tensor_tensor(out=rq[i2][:, :, 0:64], in0=qf[i2][:, :, 128:192], in1=cosb, op=OP.mult),
             r=["qf%d" % i2, "rp%d" % i2], w=["rq%d" % i2])
        S.op("pool", lambda e: e.tensor_tensor(out=rq[i2][:, :, 64:128:2], in0=qf[i2][:, :, 129:192:2], in1=sineb, op=OP.mult),
             r=["qf%d" % i2, "rp%d" % i2], w=["rq%d" % i2])
        S.op("pool", lambda e: e.tensor_tensor(out=rq[i2][:, :, 65:128:2], in0=qf[i2][:, :, 128:192:2], in1=sinob, op=OP.mult),
             r=["qf%d" % i2, "rp%d" % i2], w=["rq%d" % i2])
        S.op("pool", lambda e: e.tensor_tensor(out=qrp[i2][:], in0=rq[i2][:, :, 0:64], in1=rq[i2][:, :, 64:128], op=OP.add),
             r=["rq%d" % i2], w=["qrp%d" % i2])
        for h in range(NH):
            S.op("pe", lambda e, h=h: e.transpose(out=tpn[:, h, :], in_=qbn[i2][:, h, :], identity=ident_b[:]), r=["qbn%d" % i2], w=["tpn"])
        qrp2 = qrp[i2][:].rearrange("p (s a) d -> p s (a d)", a=2)
        for s_ in range(3):
            S.op("pe", lambda e, s_=s_: e.transpose(out=tpr[:, s_, :], in_=qrp2[:, s_, :], identity=ident_b[:]), r=["qrp%d" % i2], w=["tpr"])
        S.op("act", lambda e: e.activation(out=q_nopeT[:, :, ti * 128:(ti + 1) * 128], in_=tpn[:, 0:NH, :], func=AF.Copy), r=["tpn"], w=[("qn", ti)])
        S.op("dve", lambda e: e.tensor_copy(out=q_ropeT[:, :, ti * 128:(ti + 1) * 128], in_=tpr), r=["tpr"], w=[("qr", ti)])
    S.barrier()
    add_dbg("q_nopeT", q_nopeT[:, :, 0:256], [128, NH, 256], BF16)
    add_dbg("q_ropeT", q_ropeT[:, :, 0:256], [128, 3, 256], BF16)
    flush_dbg()
    sc2t.close()
    def sb3(name, shape, dt):
        return sc3.enter_context(nc.sbuf_tensor(name, list(shape), dt, side="right"))
    lg = sb3("lg", [128, NTO, NE], F32)
    d4i = sb3("d4i", [128, NTO * 4], I32)
    beci = sb3("beci", [128, 2 * NSLOT], I32)
    idxw = sb3("idxw", [128, NSLOT], I32)
    attT = sc2.enter_context(nc.sbuf_tensor("attT", [128, NH, NOWN], BF16, side="right"))
    if stage < 5:
        close_all(); S.barrier(); es.close(); return nc, list(dbg_out)

    if stage >= 8:
        ncast = 0
        for ex in range(NE):
            for (wsrc, wdst) in ((wa_gate, wb_gate), (wa_up, wb_up), (wa_down, wb_down)):
                S.dma("pool", "wc%d" % (ncast % 4), wdst[ex * 128:(ex + 1) * 128, :], wsrc[ex * 128:(ex + 1) * 128, :], w=[("wb", ncast)])
                ncast += 1
    with ExitStack() as pa:
        G = 3
        NG = NKT // G
        KnT = pa.enter_context(nc.sbuf_tensor("KnT", [128, NKEY], BF16))
        Vh = pa.enter_context(nc.sbuf_tensor("Vh", [128, NKT, 128], BF16))
        PT = [pa.enter_context(nc.sbuf_tensor("PT%d" % i, [128, G * 512], BF16)) for i in range(3)]
        dacc = [pa.enter_context(nc.sbuf_tensor("dacc%d" % i, [128, 512], F32)) for i in range(2)]
        ps_s = [pa.enter_context(nc.psum_tensor("ps_s%d" % i, [128, G * 512], F32)) for i in range(2)]
        ps_o = pa.enter_context(nc.psum_tensor("ps_o", [128, 512], F32))
        ps_d = pa.enter_context(nc.psum_tensor("ps_d", [128, 512], F32))
        n_heads = NH if stage >= 6 else 1
        n_qb = 4 if stage >= 6 else 1
        cnt_ev = 0
        for h in range(n_heads):
            S._wait("pe", "act", S.cnt["act"])
            S._wait("pe", "dve", S.cnt["dve"])
            nbk = 0
            for cb in range(17):
                n = 512 if cb < 16 else NKEY - 16 * 512
                bkey = "psb%d" % (nbk % 6)
                pk = ps_s[(nbk % 6) // 3][:, ((nbk % 6) % 3) * 512:((nbk % 6) % 3 + 1) * 512]
                nbk += 1
                for kc in range(2):
                    S.op("pe", lambda e, kc=kc, cb=cb, n=n, pk=pk: e.matmul(pk[:, 0:n], lhsT=wkv[:, kc, h * 256:h * 256 + 128],
                                                                    rhs=kvnT[:, kc, cb * 512:cb * 512 + n], start=(kc == 0), stop=(kc == 1)),
                         r=["wkv"], w=[bkey])
                if cnt_ev % 2 == 0:
                    S.op("act", lambda e, cb=cb, n=n, pk=pk: e.activation(out=KnT[:, cb * 512:cb * 512 + n], in_=pk[:, 0:n], func=AF.Copy),
                         r=[bkey], w=[("KnT", cb)])
                else:
                    S.op("dve", lambda e, cb=cb, n=n, pk=pk: e.tensor_copy(out=KnT[:, cb * 512:cb * 512 + n], in_=pk[:, 0:n]),
                         r=[bkey], w=[("KnT", cb)])
                cnt_ev += 1
            for g4 in range(17):
                kts = list(range(4 * g4, min(4 * g4 + 4, NKT)))
                bkey = "psb%d" % (nbk % 6)
                pk = ps_s[(nbk % 6) // 3][:, ((nbk % 6) % 3) * 512:((nbk % 6) % 3 + 1) * 512]
                nbk += 1
                for i_, kt in enumerate(kts):
                    for kc in range(2):
                        S.op("pe", lambda e, kc=kc, kt=kt, i_=i_, pk=pk: e.matmul(pk[:, i_ * 128:(i_ + 1) * 128], lhsT=kvnT[:, kc, kt * 128:(kt + 1) * 128],
                                                                          rhs=wkv[:, kc, h * 256 + 128:h * 256 + 256], start=(kc == 0), stop=(kc == 1)),
                             r=["wkv"], w=[bkey])
                nn = len(kts)
                vdst = Vh[:, kts[0]:kts[0] + nn, :].rearrange("p a d -> p (a d)")
                if cnt_ev % 2 == 0:
                    S.op("act", lambda e, pk=pk, nn=nn, vdst=vdst: e.activation(out=vdst, in_=pk[:, 0:nn * 128], func=AF.Copy),
                         r=[bkey], w=[("Vh", g4)])
                else:
                    S.op("dve", lambda e, pk=pk, nn=nn, vdst=vdst: e.tensor_copy(out=vdst, in_=pk[:, 0:nn * 128]),
                         r=[bkey], w=[("Vh", g4)])
                cnt_ev += 1
            S._wait("pe", "act", S.cnt["act"])
            S._wait("pe", "dve", S.cnt["dve"])
            hp = (h % 2) * 64
            for qb in range(n_qb):
                qs = slice(qb * 512, (qb + 1) * 512)

                def s_mm(g):
                    for i in range(G):
                        kt = g * G + i
                        ks = slice(kt * 128, (kt + 1) * 128)
                        dst = ps_s[g % 2][:, i * 512:(i + 1) * 512]
                        S.op("pe", lambda e: e.matmul(dst, lhsT=KnT[:, ks], rhs=q_nopeT[:, h, qs], start=True, stop=False),
                             r=["KnT"], w=["ps_s%d" % (g % 2)])
                    for i in range(G):
                        kt = g * G + i
                        ks = slice(kt * 128, (kt + 1) * 128)
                        dst = ps_s[g % 2][:, i * 512:(i + 1) * 512]
                        S.op("pe", lambda e: e.matmul(dst, lhsT=kropeT[hp:hp + 64, ks], rhs=q_ropeT[hp:hp + 64, h // 2, qs], start=False, stop=True),
                             r=[], w=["ps_s%d" % (g % 2)])
                s_mm(0)
                used = [False, False]
                for g in range(NG):
                    if g + 1 < NG:
                        s_mm(g + 1)
                    pk_ = "PT%d" % (g % 3)
                    S.op("act", lambda e: e.activation(out=PT[g % 3][:], in_=ps_s[g % 2][:], func=AF.Exp, scale=QSCALE),
                         r=["ps_s%d" % (g % 2)], w=[pk_])
                    for i in range(G):
                        kt = g * G + i
                        S.op("pe", lambda e: e.matmul(ps_o[:], lhsT=Vh[:, kt, :], rhs=PT[g % 3][:, i * 512:(i + 1) * 512],
                                                      start=(kt == 0), stop=(kt == NKT - 1)), r=["Vh", pk_], w=["ps_o"])
                    a_ = 0
                    en_ = "dve"
                    for i in range(G):
                        pti = PT[g % 3][:, i * 512:(i + 1) * 512]
                        if not used[a_]:
                            S.op(en_, lambda e: e.tensor_copy(out=dacc[a_][:], in_=pti), r=[pk_], w=["dacc%d" % a_])
                            used[a_] = True
                        else:
                            S.op(en_, lambda e: e.tensor_tensor(out=dacc[a_][:], in0=dacc[a_][:], in1=pti, op=OP.add),
                                 r=[pk_, "dacc%d" % a_], w=["dacc%d" % a_])
                S.op("pe", lambda e: e.matmul(ps_d[:], lhsT=ones_f[:], rhs=dacc[0][:], start=True, stop=True),
                     r=["dacc0", "ones_f"], w=["ps_d"])
                S.op("dve", lambda e: e.reciprocal(out=dacc[0][:], in_=ps_d[:]), r=["ps_d"], w=["dacc0"])
                S.op("dve", lambda e: e.tensor_tensor(out=attT[:, h, qs], in0=ps_o[:], in1=dacc[0][:], op=OP.mult), r=["ps_o", "dacc0"], w=[("attT", h, qb)])
        S.barrier()
    add_dbg("attT", attT[:], [128, NH, NOWN], BF16)
    flush_dbg()
    sc2q.close()
    if stage < 7:
        close_all(); S.barrier(); es.close(); return nc, list(dbg_out)


    with ExitStack() as p3:
        def sbp(name, shape, dt):
            return p3.enter_context(nc.sbuf_tensor("c_" + name, list(shape), dt))

        def psp(name, shape, dt):
            return p3.enter_context(nc.psum_tensor("c_" + name, list(shape), dt))
        wo = sbp("wo", [128, 8, D], BF16)
        wr = sbp("wr", [128, 8, NE], F32)
        brt = sbp("brt", [128, NE], F32)
        S.dma("pool", "wl", wo[:], w_out.rearrange("(kc p) n -> p kc n", p=128), w=["wo"])
        S.dma("sp", "c0", wr[:], w_router.rearrange("(kc p) n -> p kc n", p=128), w=["wr"])
        S.dma("sp", "c0", brt[:], b_router.partition_broadcast(128), w=["brt"])
        xo = [sbp("xo%d" % i, [128, D], F32) for i in range(2)]
        mixs = [sbp("mixs%d" % i, [128, D], F32) for i in range(2)]
        tmp3 = [sbp("tmp3%d" % i, [128, D], F32) for i in range(2)]
        hx2T = [sbp("hx2T%d" % i, [128, 8, 128], F32) for i in range(2)]
        st3 = [sbp("st3%d" % i, [128, 8], F32) for i in range(2)]
        junk3 = sbp("junk3", [128, D], BF16)
        hx2bt = [sbp("hx2bt%d" % i, [128, D], BF16) for i in range(2)]
        ps_m = [[psp("ps_m%d_%d" % (i, nb), [128, 512], F32) for nb in range(2)] for i in range(2)]
        tp_f = psp("tp_f", [128, D], F32)
        ps_l = psp("ps_l", [128, 512], F32)
        for ti in range(NTO):
            i2 = ti % 2
            cs = slice(ti * 128, (ti + 1) * 128)
            xk, mk, tk, sk = "xo%d" % i2, "mixs%d" % i2, "tmp3%d" % i2, "st3%d" % i2
            if ti == 0:
                S.dma("sp", xk, xo[i2][:], x_own[cs, :], w=[xk])
            if ti + 1 < NTO:
                S.dma("sp", "xo%d" % ((ti + 1) % 2), xo[(ti + 1) % 2][:], x_own[(ti + 1) * 128:(ti + 2) * 128, :], w=["xo%d" % ((ti + 1) % 2)])
            for nb in range(2):
                for c in range(8):
                    lt = attT[:, c, cs] if c < NH else fourT[:, c - NH, cs]
                    S.op("pe", lambda e, nb=nb, c=c, lt=lt: e.matmul(ps_m[i2][nb][:], lhsT=lt, rhs=wo[:, c, nb * 512:(nb + 1) * 512],
                                                                 start=(c == 0), stop=(c == 7)), r=["wo"], w=["ps_m%d_%d" % (i2, nb)])
                S.op("act", lambda e, nb=nb: e.activation(out=mixs[i2][:, nb * 512:(nb + 1) * 512], in_=ps_m[i2][nb][:], func=AF.Copy),
                     r=["ps_m%d_%d" % (i2, nb)], w=[mk])
            S.op("act", lambda e: e.activation(out=junk3[:], in_=mixs[i2][:], func=AF.Square, scale=1.0 / 32.0, accum_out=st3[i2][:, 0:1]),
                 r=[mk], w=["junk3", sk])
            rstd(st3[i2], 0, 1, sk)
            S.op("dve", lambda e: e.scalar_tensor_tensor(out=tmp3[i2][:], in0=mixs[i2][:], scalar=st3[i2][:, 1:2], in1=mod["G1"][:], op0=OP.mult, op1=OP.mult),
                 r=[mk, sk], w=[tk])
            S.op("pool", lambda e: e.tensor_tensor(out=xo[i2][:], in0=tmp3[i2][:], in1=xo[i2][:], op=OP.add), r=[tk, xk], w=[xk])
            S.dma("sp", "x1st", x1_d[cs, :], xo[i2][:], r=[xk], w=[("x1d", ti)])
            S.op("act", lambda e: e.activation(out=junk3[:], in_=xo[i2][:], func=AF.Square, scale=1.0 / 32.0, accum_out=st3[i2][:, 2:3]),
                 r=[xk], w=["junk3", sk])
            rstd(st3[i2], 2, 3, sk)
            S.op("dve", lambda e: e.scalar_tensor_tensor(out=tmp3[i2][:], in0=xo[i2][:], scalar=st3[i2][:, 3:4], in1=mod["A2"][:], op0=OP.mult, op1=OP.mult),
                 r=[xk, sk], w=[tk])
            S.op("pool", lambda e: e.tensor_tensor(out=mixs[i2][:], in0=tmp3[i2][:], in1=mod["B2"][:], op=OP.add), r=[tk], w=[mk])
            S.op("pool", lambda e: e.tensor_copy(out=hx2bt[i2][:], in_=mixs[i2][:]), r=[mk], w=["hx2bt%d" % i2])
            S.dma("sp", "h2st", h2_d[cs, :], hx2bt[i2][:], r=["hx2bt%d" % i2], w=[("h2d", ti)])
            for kc in range(8):
                S.op("pe", lambda e, kc=kc: e.transpose(out=tp_f[:, kc * 128:(kc + 1) * 128], in_=mixs[i2][:, kc * 128:(kc + 1) * 128], identity=ident_f[:]),
                     r=[mk], w=["tp_f"])
            S.op("act", lambda e: e.activation(out=hx2T[i2][:].rearrange("p a b -> p (a b)"), in_=tp_f[:], func=AF.Copy), r=["tp_f"], w=["hx2T%d" % i2])
            for kc in range(8):
                S.op("pe", lambda e, kc=kc: e.matmul(ps_l[:, 0:NE], lhsT=hx2T[i2][:, kc, :], rhs=wr[:, kc, :], start=(kc == 0), stop=(kc == 7)),
                     r=["hx2T%d" % i2, "wr"], w=["ps_l"])
            S.op("dve", lambda e: e.tensor_tensor(out=lg[:, ti, :], in0=ps_l[:, 0:NE], in1=brt[:], op=OP.add), r=["ps_l", "brt"], w=[("lg", ti)])
        S.barrier()
        add_dbg("lg", lg[:], [128, NTO, NE], F32)
        flush_dbg()
    sc2.close()
    scA.close()

    with ExitStack() as pr_:
        def sbr(name, shape, dt):
            return pr_.enter_context(nc.sbuf_tensor("r_" + name, list(shape), dt))
        t8 = sbr("t8", [128, NTO, 8], F32)
        mask = sbr("mask", [128, NTO, NE], F32)
        negmax = sbr("negmax", [128, NTO], F32)
        ex = sbr("ex", [128, NTO, NE], F32)
        den = sbr("den", [128, NTO], F32)
        gate = sbr("gate", [128, NTO, NE], F32)
        keyt = sbr("keyt", [128, NE], F32)
        k8 = sbr("k8", [128, NTO, 8], F32)
        maskb = sbr("maskb", [128, NTO, NE], BF16)
        iota1 = sbr("iota1", [128, NE + NSLOT + 1], F32)
        tri_f = sbr("tri_f", [128, 128], F32)
        tri_b = sbr("tri_b", [128, 128], BF16)
        uu = sbr("uu", [NE, 2 * NE], F32)
        nbf = sbr("nbf", [NE, 128], F32)
        nbi = sbr("nbi", [NE, 128], I32)
        blk = sbr("blk", [128, 2 * NE], F32)
        dest = sbr("dest", [128, NTO, NE], F32)
        oh = [sbr("oh%d" % i, [128, NE], F32) for i in range(2)]
        pr1 = [sbr("pr1%d" % i, [128, NE], F32) for i in range(2)]
        pr2 = [sbr("pr2%d" % i, [128, NE], F32) for i in range(2)]
        d4f = sbr("d4f", [128, NTO * 4], F32)
        g4 = sbr("g4", [128, NTO * 4], F32)
        cmp_ = sbr("cmp", [128, NSLOT, NE], F32)
        bef = sbr("bef", [128, 2 * NSLOT], F32)
        idxf = sbr("idxf", [128, NSLOT], F32)
        p_t = pr_.enter_context(nc.psum_tensor("p_t", [128, 512], F32))
        p_b = pr_.enter_context(nc.psum_tensor("p_b", [128, 512], F32))
        p_r = pr_.enter_context(nc.psum_tensor("p_r", [128, 512], F32))
        S.dma("sp", "c0", iota1[:, 0:NE], iota_d[:, 0:NE], w=["iota1"])
        S.dma("sp", "c0", iota1[:, NE:NE + NSLOT + 1], iota_d[:, NE:NE + NSLOT + 1], w=["iota1"])
        S.dma("sp", "c0", tri_f[:], tri_d, w=["tri_f"])
        S.dma("sp", "c0", uu[:], uu_d, w=["uu"])
        S.op("dve", lambda e: e.tensor_copy(out=tri_b[:], in_=tri_f[:]), r=["tri_f"], w=["tri_b"])
        for ti in range(NTO):
            S.op("dve", lambda e: e.max(out=t8[:, ti, :], in_=lg[:, ti, :]), r=[], w=["t8"])
        S.op("dve", lambda e: e.tensor_scalar(out=negmax[:], in0=t8[:, :, 0], scalar1=-1.0, scalar2=None, op0=OP.mult), r=["t8"], w=["negmax"])
        for ti in range(NTO):
            S.op("dve", lambda e: e.tensor_scalar(out=mask[:, ti, :], in0=lg[:, ti, :], scalar1=t8[:, ti, 3:4], scalar2=None, op0=OP.is_ge),
                 r=["t8"], w=["mask"])
            S.op("act", lambda e: e.activation(out=ex[:, ti, :], in_=lg[:, ti, :], func=AF.Exp, bias=negmax[:, ti:ti + 1], scale=1.0),
                 r=["negmax"], w=["ex"])
        S.op("dve", lambda e: e.tensor_tensor(out=ex[:], in0=ex[:], in1=mask[:], op=OP.mult), r=["ex", "mask"], w=["ex"])
        S.op("dve", lambda e: e.reduce_sum(out=den[:], in_=ex[:], axis=AX.X), r=["ex"], w=["den"])
        S.op("dve", lambda e: e.reciprocal(out=den[:], in_=den[:]), r=["den"], w=["den"])
        for ti in range(NTO):
            S.op("dve", lambda e: e.tensor_scalar(out=gate[:, ti, :], in0=ex[:, ti, :], scalar1=den[:, ti:ti + 1], scalar2=None, op0=OP.mult),
                 r=["ex", "den"], w=["gate"])
            S.op("dve", lambda e: e.tensor_tensor(out=keyt[:], in0=mask[:, ti, :], in1=iota1[:, 0:NE], op=OP.mult), r=["mask", "iota1"], w=["keyt"])
            S.op("dve", lambda e: e.max(out=k8[:, ti, :], in_=keyt[:]), r=["keyt"], w=["k8"])
        S.op("dve", lambda e: e.tensor_copy(out=maskb[:], in_=mask[:]), r=["mask"], w=["maskb"])
        for ti in range(NTO):
            S.op("pe", lambda e: e.matmul(p_t[0:NE, 0:128], lhsT=maskb[:, ti, :], rhs=ones_b[:], start=(ti == 0), stop=(ti == NTO - 1)),
                 r=["maskb", "ones_b"], w=["p_t"])
        S.op("dve", lambda e: e.tensor_scalar(out=nbf[:], in0=p_t[0:NE, 0:128], scalar1=127.0, scalar2=None, op0=OP.add), r=["p_t"], w=["nbf"])
        S.op("dve", lambda e: e.tensor_copy(out=nbi[:], in_=nbf[:]), r=["nbf"], w=["nbi"])
        S.op("dve", lambda e: e.tensor_single_scalar(out=nbi[:], in_=nbi[:], scalar=7, op=OP.arith_shift_right), r=["nbi"], w=["nbi"])
        S.op("dve", lambda e: e.tensor_copy(out=nbf[:], in_=nbi[:]), r=["nbi"], w=["nbf"])
        S.op("pe", lambda e: e.matmul(p_b[:, 0:2 * NE], lhsT=nbf[:], rhs=uu[:], start=True, stop=True), r=["nbf", "uu"], w=["p_b"])
        S.op("dve", lambda e: e.tensor_copy(out=blk[:], in_=p_b[:, 0:2 * NE]), r=["p_b"], w=["blk"])
        for ti in range(NTO):
            for tp_ in range(ti):
                S.op("pe", lambda e, tp_=tp_: e.matmul(p_r[:, ti * NE:(ti + 1) * NE], lhsT=ones_b[:], rhs=maskb[:, tp_, :], start=(tp_ == 0), stop=False),
                     r=["maskb"], w=["p_r"])
            S.op("pe", lambda e: e.matmul(p_r[:, ti * NE:(ti + 1) * NE], lhsT=tri_b[:], rhs=maskb[:, ti, :], start=(ti == 0), stop=True),
                 r=["maskb", "tri_b"], w=["p_r"])
        for ti in range(NTO):
            S.op("dve", lambda e: e.scalar_tensor_tensor(out=dest[:, ti, :], in0=blk[:, 0:NE], scalar=128.0, in1=p_r[:, ti * NE:(ti + 1) * NE],
                                                         op0=OP.mult, op1=OP.add), r=["blk", "p_r"], w=["dest"])
        oh3 = sbr("oh3", [128, NTO, NE], F32)
        pr3 = sbr("pr3", [128, NTO, NE], F32)
        d4v = d4f[:].rearrange("p (t k) -> p t k", k=4)
        g4v = g4[:].rearrange("p (t k) -> p t k", k=4)
        for k in range(4):
            S.op("dve", lambda e: e.tensor_tensor(out=oh3[:], in0=iota1[:, 0:NE].unsqueeze(1).to_broadcast([128, NTO, NE]),
                                                  in1=k8[:, :, k:k + 1].to_broadcast([128, NTO, NE]), op=OP.is_equal), r=["k8", "iota1"], w=["oh3"])
            S.op("dve", lambda e: e.tensor_tensor(out=pr3[:], in0=oh3[:], in1=dest[:], op=OP.mult), r=["oh3", "dest"], w=["pr3"])
            S.op("dve", lambda e: e.reduce_sum(out=d4v[:, :, k], in_=pr3[:], axis=AX.X), r=["pr3"], w=["d4f"])
            S.op("dve", lambda e: e.tensor_tensor(out=pr3[:], in0=oh3[:], in1=gate[:], op=OP.mult), r=["oh3", "gate"], w=["pr3"])
            S.op("dve", lambda e: e.reduce_sum(out=g4v[:, :, k], in_=pr3[:], axis=AX.X), r=["pr3"], w=["g4"])
        S.op("dve", lambda e: e.tensor_copy(out=d4i[:], in_=d4f[:]), r=["d4f"], w=["d4i"])
        S.op("dve", lambda e: e.tensor_tensor(out=cmp_[:], in0=blk[:, NE:2 * NE].unsqueeze(1).to_broadcast([128, NSLOT, NE]),
                                              in1=iota1[:, NE:NE + NSLOT].unsqueeze(2).to_broadcast([128, NSLOT, NE]), op=OP.is_le),
             r=["blk", "iota1"], w=["cmp"])
        S.op("dve", lambda e: e.reduce_sum(out=bef[:, 0:NSLOT], in_=cmp_[:], axis=AX.X), r=["cmp"], w=["bef"])
        S.op("dve", lambda e: e.tensor_scalar(out=bef[:, 0:NSLOT], in0=bef[:, 0:NSLOT], scalar1=float(NE - 1), scalar2=None, op0=OP.min), r=["bef"], w=["bef"])
        S.op("dve", lambda e: e.memset(bef[:, NSLOT:NSLOT + 2], 1.0), r=[], w=["bef"])
        S.op("dve", lambda e: e.tensor_tensor(out=bef[:, NSLOT + 2:2 * NSLOT], in0=bef[:, 2:NSLOT], in1=bef[:, 0:NSLOT - 2], op=OP.not_equal), r=["bef"], w=["bef"])
        S.op("dve", lambda e: e.tensor_copy(out=beci[:], in_=bef[:]), r=["bef"], w=["beci"])
        BIG = 1000000.0
        S.op("dve", lambda e: e.scalar_tensor_tensor(out=idxf[:], in0=bef[:, 0:NSLOT], scalar=128.0,
                                                     in1=iota1[:, NE + NSLOT:NE + NSLOT + 1].to_broadcast([128, NSLOT]), op0=OP.mult, op1=OP.add),
             r=["bef", "iota1"], w=["idxf"])
        S.op("dve", lambda e: e.tensor_scalar(out=idxf[:], in0=idxf[:], scalar1=-BIG, scalar2=None, op0=OP.add), r=["idxf"], w=["idxf"])
        S.op("dve", lambda e: e.tensor_tensor(out=idxf[:], in0=idxf[:], in1=bef[:, NSLOT:2 * NSLOT], op=OP.mult), r=["idxf", "bef"], w=["idxf"])
        S.op("dve", lambda e: e.tensor_scalar(out=idxf[:], in0=idxf[:], scalar1=BIG, scalar2=None, op0=OP.add), r=["idxf"], w=["idxf"])
        S.op("dve", lambda e: e.tensor_copy(out=idxw[:], in_=idxf[:]), r=["idxf"], w=["idxw"])
        S.barrier()
        add_dbg("d4i", d4i[:], [128, NTO * 4], I32)
        add_dbg("g4", g4[:], [128, NTO * 4], F32)
        add_dbg("beci", beci[0:1, :], [1, 2 * NSLOT], I32)
        add_dbg("idxw", idxw[:], [128, NSLOT], I32)
        flush_dbg()
        hx2r = [[sbr("hx2r%d_%d" % (i, k), [128, D + 32], BF16) for k in range(4)] for i in range(2)]
        ghl = sbr("ghl", [128, NTO * 4, 2], BF16)
        gres = sbr("gres", [128, NTO * 4], F32)
        S.op("dve", lambda e: e.tensor_copy(out=ghl[:, :, 0], in_=g4[:]), r=["g4"], w=["ghl"])
        S.op("dve", lambda e: e.tensor_tensor(out=gres[:], in0=g4[:], in1=ghl[:, :, 0], op=OP.subtract), r=["g4", "ghl"], w=["gres"])
        S.op("dve", lambda e: e.tensor_copy(out=ghl[:, :, 1], in_=gres[:]), r=["gres"], w=["ghl"])
        for i_ in range(2):
            for k_ in range(4):
                S.op("dve", lambda e: e.memset(hx2r[i_][k_][:, D:D + 32], 0.0), w=["hx2r%d_%d" % (i_, k_)])
        for ti in range(NTO):
            i2 = ti % 2
            for k in range(4):
                col = ti * 4 + k
                hk = "hx2r%d_%d" % (i2, k)
                S.dma("sp", "h2ld" + hk, hx2r[i2][k][:, 0:D], h2_d[ti * 128:(ti + 1) * 128, :], w=[hk])
                S.op("dve", lambda e: e.tensor_copy(out=hx2r[i2][k][:, D:D + 2], in_=ghl[:, col, :]), r=["ghl"], w=[hk])
                S.idma("sc_x%d" % k, r=[hk], w=["xs_d"], out=xs_d, out_offset=bass.IndirectOffsetOnAxis(ap=d4i[:, col:col + 1], axis=0),
                       in_=hx2r[i2][k][:], in_offset=None)
        S.barrier()
    if stage < 8:
        close_all(); S.barrier(); es.close(); return nc, list(dbg_out)

    n_slots = NSLOT if stage >= 9 else 4
    with ExitStack() as pm_:
        def sbm(name, shape, dt):
            return pm_.enter_context(nc.sbuf_tensor("m_" + name, list(shape), dt))

        def psm(name, shape, dt):
            return pm_.enter_context(nc.psum_tensor("m_" + name, list(shape), dt))
        wg = [sbm("wg%d" % i, [128, 9, D], BF16) for i in range(2)]
        wu_ = [sbm("wu%d" % i, [128, 9, D], BF16) for i in range(2)]
        wd = [sbm("wd%d" % i, [128, 9, D], BF16) for i in range(2)]
        bg = [wg[i][:, 8, :] for i in range(2)]
        bu = [wu_[i][:, 8, :] for i in range(2)]
        bd = [wd[i][:, 8, :] for i in range(2)]
        xsb = [sbm("xsb%d" % i, [128, D + 32], BF16) for i in range(2)]
        xT = [sbm("xT%d" % i, [128, 8, 128], BF16) for i in range(2)]
        gm = sbm("gm", [128, D], F32)
        sg = sbm("sg", [128, D], F32)
        u1 = sbm("u1", [128, D], F32)
        actb = sbm("actb", [128, D], BF16)
        actT = sbm("actT", [128, 8, 128], BF16)
        ysb = [sbm("ysb%d" % i, [128, D], F32) for i in range(2)]
        gsc = [sbm("gsc%d" % i, [128, 1], F32) for i in range(2)]
        tpx = psm("tpx", [128, 8, 128], BF16)
        psg = psm("psg", [128, D], F32)
        psu = psm("psu", [128, D], F32)
        tpa = psm("tpa", [128, 8, 128], BF16)
        psy = psm("psy", [128, D], F32)
        rb_ = pm_.enter_context(nc.gpsimd.register("rb_"))
        nc.gpsimd.reg_mov(rb_, NE * 128 - 1)
        bv = nc.gpsimd.snap(rb_)

        def load_w(b):
            p = b % 2
            for (wt, wsrc, key) in ((wg[p], wb_gate, "wg%d" % p), (wu_[p], wb_up, "wu%d" % p), (wd[p], wb_down, "wd%d" % p)):
                S.idma("wld%d_%s" % (p, key), r=[], w=[key], out=wt[:].rearrange("p a n -> p (a n)"), out_offset=None, in_=wsrc,
                       in_offset=bass.IndirectOffsetOnAxis(ap=idxw[:, b:b + 1], axis=0), bounds_check=bv, oob_is_err=False)

        load_w(0)
        for b in range(n_slots):
            p = b % 2
            if b + 1 < n_slots:
                load_w(b + 1)
            rs = slice(b * 128, (b + 1) * 128)
            if b == 0:
                S.dma("sp", "xsl%d" % p, xsb[p][:], xs_d[rs, :], w=["xsb%d" % p])
            for kc in range(8):
                S.op("pe", lambda e, kc=kc: e.transpose(out=tpx[:, kc, :], in_=xsb[p][:, kc * 128:(kc + 1) * 128], identity=ident_b[:]),
                     r=["xsb%d" % p], w=["tpx"])
            S.op("act", lambda e: e.activation(out=xT[p][:], in_=tpx[:], func=AF.Copy), r=["tpx"], w=["xT%d" % p])
            for (pt, wt, bt, pk, wk) in ((psg, wg[p], bg[p], "psg", "wg%d" % p), (psu, wu_[p], bu[p], "psu", "wu%d" % p)):
                for hf in range(2):
                    hs = slice(hf * 512, (hf + 1) * 512)
                    for kc in range(8):
                        S.op("pe", lambda e, kc=kc, pt=pt, wt=wt, hs=hs: e.matmul(pt[:, hs], lhsT=xT[p][:, kc, :], rhs=wt[:, kc, hs], start=(kc == 0), stop=False),
                             r=["xT%d" % p, wk], w=[pk])
                    S.op("pe", lambda e, pt=pt, bt=bt, hs=hs: e.matmul(pt[:, hs], lhsT=ones_b[0:1, :], rhs=bt[0:1, hs], start=False, stop=True),
                         r=[wk], w=[pk])
            if b + 1 < n_slots:
                S.dma("sp", "xsl%d" % ((b + 1) % 2), xsb[(b + 1) % 2][:], xs_d[(b + 1) * 128:(b + 2) * 128, :], w=["xsb%d" % ((b + 1) % 2)])
            S.op("dve", lambda e: e.tensor_scalar(out=gm[:], in0=psg[:], scalar1=7.0, scalar2=None, op0=OP.min), r=["psg"], w=["gm"])
            S.op("act", lambda e: e.activation(out=sg[:], in_=gm[:], func=AF.Sigmoid, scale=1.702), r=["gm"], w=["sg"])
            S.op("dve", lambda e: e.tensor_scalar(out=u1[:], in0=psu[:], scalar1=7.0, scalar2=-7.0, op0=OP.min, op1=OP.max), r=["psu"], w=["u1"])
            S.op("dve", lambda e: e.scalar_tensor_tensor(out=u1[:], in0=u1[:], scalar=1.0, in1=gm[:], op0=OP.add, op1=OP.mult), r=["u1", "gm"], w=["u1"])
            S.op("dve", lambda e: e.tensor_tensor(out=actb[:], in0=u1[:], in1=sg[:], op=OP.mult), r=["u1", "sg"], w=["actb"])
            for kc in range(8):
                S.op("pe", lambda e, kc=kc: e.transpose(out=tpa[:, kc, :], in_=actb[:, kc * 128:(kc + 1) * 128], identity=ident_b[:]),
                     r=["actb"], w=["tpa"])
            S.op("act", lambda e: e.activation(out=actT[:], in_=tpa[:], func=AF.Copy), r=["tpa"], w=["actT"])
            for hf in range(2):
                hs = slice(hf * 512, (hf + 1) * 512)
                for kc in range(8):
                    S.op("pe", lambda e, kc=kc, hs=hs: e.matmul(psy[:, hs], lhsT=actT[:, kc, :], rhs=wd[p][:, kc, hs], start=(kc == 0), stop=False),
                         r=["actT", "wd%d" % p], w=["psy"])
                S.op("pe", lambda e, hs=hs: e.matmul(psy[:, hs], lhsT=ones_b[0:1, :], rhs=bd[p][0:1, hs], start=False, stop=True),
                     r=["wd%d" % p], w=["psy"])
            S.op("dve", lambda e: e.tensor_tensor(out=gsc[p][:], in0=xsb[p][:, D:D + 1], in1=xsb[p][:, D + 1:D + 2], op=OP.add), r=["xsb%d" % p], w=["gsc%d" % p])
            S.op("act", lambda e: e.activation(out=ysb[p][:], in_=psy[:], func=AF.Copy, scale=gsc[p][:, 0:1]), r=["psy", "gsc%d" % p], w=["ysb%d" % p])
            S.dma("sp", "yst%d" % p, ys_d[rs, :], ysb[p][:], r=["ysb%d" % p], w=["ys_d"])
        S.barrier()

    with ExitStack() as pc_:
        def sbc_(name, shape, dt):
            return pc_.enter_context(nc.sbuf_tensor("f_" + name, list(shape), dt))
        yg = [[sbc_("yg%d_%d" % (i, k), [128, D], F32) for k in range(4)] for i in range(2)]
        x1r = [sbc_("x1r%d" % i, [128, D], F32) for i in range(2)]
        st4 = [sbc_("st4%d" % i, [128, 8], F32) for i in range(2)]
        junk4 = sbc_("junk4", [128, D], BF16)
        for ti in range(NTO):
            i2 = ti % 2
            cs = slice(ti * 128, (ti + 1) * 128)
            def fetch(t_):
                j2 = t_ % 2
                S.dma("sp", "x1r%d" % j2, x1r[j2][:], x1_d[t_ * 128:(t_ + 1) * 128, :], w=["x1r%d" % j2])
                for k in range(4):
                    col = t_ * 4 + k
                    S.idma("yg%d_%d" % (j2, k), r=[], w=["yg%d_%d" % (j2, k)], out=yg[j2][k][:], out_offset=None, in_=ys_d,
                           in_offset=bass.IndirectOffsetOnAxis(ap=d4i[:, col:col + 1], axis=0))
            if ti == 0:
                fetch(0)
            if ti + 1 < NTO:
                fetch(ti + 1)
            S.op("dve", lambda e: e.tensor_tensor(out=yg[i2][0][:], in0=yg[i2][0][:], in1=yg[i2][1][:], op=OP.add),
                 r=["yg%d_0" % i2, "yg%d_1" % i2], w=["yg%d_0" % i2])
            S.op("dve", lambda e: e.tensor_tensor(out=yg[i2][2][:], in0=yg[i2][2][:], in1=yg[i2][3][:], op=OP.add),
                 r=["yg%d_2" % i2, "yg%d_3" % i2], w=["yg%d_2" % i2])
            S.op("dve", lambda e: e.tensor_tensor(out=yg[i2][0][:], in0=yg[i2][0][:], in1=yg[i2][2][:], op=OP.add),
                 r=["yg%d_0" % i2, "yg%d_2" % i2], w=["yg%d_0" % i2])
            S.op("act", lambda e: e.activation(out=junk4[:], in_=yg[i2][0][:], func=AF.Square, scale=1.0 / 32.0, accum_out=st4[i2][:, 0:1]),
                 r=["yg%d_0" % i2], w=["junk4", "st4%d" % i2])
            rstd(st4[i2], 0, 1, "st4%d" % i2)
            S.op("dve", lambda e: e.scalar_tensor_tensor(out=yg[i2][1][:], in0=yg[i2][0][:], scalar=st4[i2][:, 1:2], in1=mod["G2"][:], op0=OP.mult, op1=OP.mult),
                 r=["yg%d_0" % i2, "st4%d" % i2], w=["yg%d_1" % i2])
            S.op("dve", lambda e: e.tensor_tensor(out=yg[i2][1][:], in0=yg[i2][1][:], in1=x1r[i2][:], op=OP.add),
                 r=["yg%d_1" % i2, "x1r%d" % i2], w=["yg%d_1" % i2])
            S.dma("sp", "ost%d" % i2, out_d[cs, :], yg[i2][1][:], r=["yg%d_1" % i2], w=[("out", ti)])
        S.barrier()

    close_all()
    flush_dbg()
    S.barrier()
    es.close()
    return nc, list(dbg_out)


def _host_consts(j):
    half = 32
    inv = 1.0 / (10000.0 ** (np.arange(0, half, 2, dtype=np.float32) / half))

    def rope_tab(tok):
        row = (tok // 64).astype(np.float32)
        col = (tok % 64).astype(np.float32)
        ang = np.concatenate([row[:, None] * inv, col[:, None] * inv], axis=-1).astype(np.float32)
        cos, sin = np.cos(ang), np.sin(ang)
        cos2 = np.repeat(cos, 2, axis=1)
        sin2 = np.stack([-sin, sin], axis=-1).reshape(len(tok), 64)
        return np.ascontiguousarray(np.concatenate([cos2, sin2], axis=1).astype(np.float32))

    tok_all = np.arange(SEQ)
    tok_own = np.arange(j, SEQ, 4)
    lut_c = np.cos(2 * np.pi * np.arange(SEQ) / SEQ)
    lut_s = -np.sin(2 * np.pi * np.arange(SEQ) / SEQ)
    nk = (tok_all[:, None].astype(np.int64) * tok_own[None, :].astype(np.int64)) % SEQ
    dft_c = lut_c[nk].astype(ml_dtypes.bfloat16)
    dft_s = lut_s[nk].astype(ml_dtypes.bfloat16)
    cc = np.arange(64)
    ph = 2 * np.pi * np.outer(cc, cc) / 64.0
    sc = 1.0 / np.sqrt(SEQ * 64.0)
    Cc = np.cos(ph) * sc
    Sc = np.sin(ph) * sc
    cbd = np.zeros((256, 512), np.float32)
    for g in range(4):
        cbd[g * 64:(g + 1) * 64, g * 64:(g + 1) * 64] = Cc
        cbd[g * 64:(g + 1) * 64, 256 + g * 64:256 + (g + 1) * 64] = Sc
    p = np.arange(128)
    tri = (p[:, None] < p[None, :]).astype(np.float32)
    e = np.arange(NE)
    uu = np.concatenate([(e[:, None] < e[None, :]), (e[:, None] <= e[None, :])], axis=1).astype(np.float32)
    return {
        "rope_all": rope_tab(tok_all), "rope_own": rope_tab(tok_own),
        "dft_c": dft_c, "dft_s": dft_s, "cbd": cbd,
        "ident_d": np.eye(128, dtype=np.float32), "tri_d": tri,
        "iota_d": np.concatenate([np.tile(np.concatenate([np.arange(1, NE + 1), np.arange(NSLOT)]).astype(np.float32)[None, :], (128, 1)),
                                  np.arange(128, dtype=np.float32)[:, None]], axis=1),
        "uu_d": uu,
    }


_CONST_CACHE = {}


def _relayout(w, b):
    w = np.asarray(w, dtype=np.float32).reshape(NE, 8, 128, D).transpose(0, 2, 1, 3).reshape(NE, 128, 8 * D)
    bb = np.broadcast_to(np.asarray(b, dtype=np.float32).reshape(NE, 1, D), (NE, 128, D))
    return np.ascontiguousarray(np.concatenate([w, bb], axis=2).reshape(NE * 128, 9 * D))


def make_in_maps(inp):
    f = lambda a: np.ascontiguousarray(np.asarray(a, dtype=np.float32))
    x = f(inp["x"]); c = f(inp["c"]); ctx = f(inp["ctx"]); c_ctx = f(inp["c_ctx"])
    shared = {
        "cc_col": np.ascontiguousarray(c_ctx.reshape(8, 128).T),
        "w_mod": f(inp["w_mod"][0]), "b_mod": f(inp["b_mod"][0]).reshape(1, -1),
        "g_attn_pre": f(inp["norm_attn_pre"][0]).reshape(1, -1), "g_attn_post": f(inp["norm_attn_post"][0]).reshape(1, -1),
        "g_ffn_pre": f(inp["norm_ffn_pre"][0]).reshape(1, -1), "g_ffn_post": f(inp["norm_ffn_post"][0]).reshape(1, -1),
        "w_in": f(inp["w_in"][0]), "g_q": f(inp["norm_q_lat"][0]).reshape(1, -1), "g_kv": f(inp["norm_kv_lat"][0]).reshape(1, -1),
        "w_q_up": f(inp["w_q_up"][0]), "w_kv_up": f(inp["w_kv_up"][0]), "w_four": f(inp["w_fourier"][0]).reshape(256, 64),
        "w_out": f(inp["w_out"][0]), "w_router": f(inp["w_router"][0]), "b_router": f(inp["b_router"][0]).reshape(1, -1),
        "wa_gate": _relayout(inp["w_gate"][0], inp["b_gate"][0]),
        "wa_up": _relayout(inp["w_up"][0], inp["b_up"][0]),
        "wa_down": _relayout(inp["w_down"][0], inp["b_down"][0]),
    }
    maps = []
    for i in range(8):
        b, j = i // 4, i % 4
        if j not in _CONST_CACHE:
            _CONST_CACHE[j] = _host_consts(j)
        m = dict(shared)
        m.update(_CONST_CACHE[j])
        m["x_b"] = x[b]
        m["x_own"] = np.ascontiguousarray(x[b, j::4])
        m["ctx_b"] = ctx[b]
        m["c_col"] = np.ascontiguousarray(c[b].reshape(8, 128).T)
        maps.append(m)
    return maps


_NC_CACHE = {}


def kernel(**inputs):
    if "nc" not in _NC_CACHE:
        _NC_CACHE["nc"] = build_nc()[0]
    nc = _NC_CACHE["nc"]
    maps = make_in_maps(inputs)
    res = run_bass_kernel_spmd(nc, maps, core_ids=list(range(8)))
    out = np.empty((2, SEQ, D), np.float32)
    for i in range(8):
        b, j = i // 4, i % 4
        out[b, j::4] = res.results[i]["out"]
    return out
```

```python
from contextlib import ExitStack
import numpy as np
import ml_dtypes
import concourse.bass as bass
import concourse.mybir as mybir
from concourse.bass_utils import run_bass_kernel_spmd

F32 = mybir.dt.float32
BF16 = mybir.dt.bfloat16
I32 = mybir.dt.int32
AF = mybir.ActivationFunctionType
OP = mybir.AluOpType
AX = mybir.AxisListType

D = 1024
SEQ = 8192
CTXL = 256
NKT = (SEQ + CTXL) // 128
NKEY = SEQ + CTXL
NOWN = 2048
NTO = NOWN // 128
NH = 6
NE = 32
NSLOT = 96
EPS = 1e-6
QSCALE = 192.0 ** -0.5


class Sch:
    def __init__(self, nc, es):
        self.nc = nc
        self.es = es
        self.eng = {"pe": nc.tensor, "act": nc.scalar, "dve": nc.vector, "pool": nc.gpsimd, "sp": nc.sync}
        self.sem = {}
        self.cnt = {}
        for k in self.eng:
            self.sem[k] = es.enter_context(nc.semaphore("sem_" + k))
            self.cnt[k] = 0
        self.waited = {k: {} for k in self.eng}
        self.last_w = {}
        self.readers = {}
        self.n2p = {}

    def dsem(self, name, sw=False):
        name = ("G:" if sw else "H:") + name
        if name not in self.n2p:
            pk = ("DG%d" if sw else "DH%d") % sum(1 for k in self.n2p if k.startswith(name[:2]))
            if pk not in self.sem:
                self.sem[pk] = self.es.enter_context(self.nc.semaphore("dsem_" + pk))
                self.cnt[pk] = 0
            self.n2p[name] = pk
        return self.n2p[name]

    def _wait(self, e, s, v):
        if v <= 0:
            return
        if s == e and e == "pe":
            return
        if s.startswith("D"):
            v = max(v, self.cnt[s])
        if self.waited[e].get(s, 0) >= v:
            return
        self.eng[e].wait_ge(self.sem[s], v)
        self.waited[e][s] = v

    def _deps(self, e, r, w):
        for k in r:
            ev = self.last_w.get(k)
            if ev:
                self._wait(e, *ev)
        for k in w:
            ev = self.last_w.get(k)
            if ev:
                self._wait(e, *ev)
            for ev in self.readers.get(k, ()):
                self._wait(e, *ev)

    def _record(self, me, r, w):
        for k in r:
            self.readers.setdefault(k, []).append(me)
        for k in w:
            self.last_w[k] = me
            self.readers[k] = []

    def op(self, e, fn, r=(), w=()):
        self._deps(e, r, w)
        ins = fn(self.eng[e])
        self.cnt[e] += 1
        ins.then_inc(self.sem[e], 1)
        self._record((e, self.cnt[e]), r, w)
        return ins

    def dma(self, q, sname, out, in_, r=(), w=(), **kw):
        sname = self.dsem(sname, sw=(q == "pool"))
        self._deps(q, r, w)
        self._wait(q, sname, self.cnt[sname])
        ins = self.eng[q].dma_start(out=out, in_=in_, **kw)
        self.cnt[sname] += 16
        ins.then_inc(self.sem[sname], 16)
        self._record((sname, self.cnt[sname]), r, w)
        return ins

    def idma(self, sname, r=(), w=(), **kw):
        sname = self.dsem(sname, sw=True)
        self._deps("pool", r, w)
        self._wait("pool", sname, self.cnt[sname])
        ins = self.nc.gpsimd.indirect_dma_start(**kw)
        self.cnt[sname] += 16
        ins.then_inc(self.sem[sname], 16)
        self._record((sname, self.cnt[sname]), r, w)
        return ins

    def barrier(self):
        for e in self.eng:
            for s in list(self.sem):
                if s != e:
                    self._wait(e, s, self.cnt[s])
        self.last_w = {}
        self.readers = {}
        self.n2p = {}


def build_nc(stage=99, dbg=()):
    nc = bass.Bass("TRN2", target_bir_lowering=False)
    es = ExitStack()

    def dram(name, shape, dt, kind="ExternalInput"):
        return nc.dram_tensor(name, list(shape), dt, kind=kind).ap()

    x_b = dram("x_b", [SEQ, D], F32)
    x_own = dram("x_own", [NOWN, D], F32)
    ctx_b = dram("ctx_b", [CTXL, D], F32)
    c_col = dram("c_col", [128, 8], F32)
    cc_col = dram("cc_col", [128, 8], F32)
    w_mod = dram("w_mod", [D, 6 * D], F32)
    b_mod = dram("b_mod", [1, 6 * D], F32)
    g_attn_pre = dram("g_attn_pre", [1, D], F32)
    g_attn_post = dram("g_attn_post", [1, D], F32)
    g_ffn_pre = dram("g_ffn_pre", [1, D], F32)
    g_ffn_post = dram("g_ffn_post", [1, D], F32)
    w_in = dram("w_in", [D, 960], F32)
    g_q = dram("g_q", [1, 384], F32)
    g_kv = dram("g_kv", [1, 256], F32)
    w_q_up = dram("w_q_up", [384, 1152], F32)
    w_kv_up = dram("w_kv_up", [256, 1536], F32)
    w_four = dram("w_four", [256, 64], F32)
    w_out = dram("w_out", [D, D], F32)
    w_router = dram("w_router", [D, NE], F32)
    b_router = dram("b_router", [1, NE], F32)
    wa_gate = dram("wa_gate", [NE * 128, 9 * D], F32)
    wa_up = dram("wa_up", [NE * 128, 9 * D], F32)
    wa_down = dram("wa_down", [NE * 128, 9 * D], F32)
    wb_gate = dram("wb_gate", [NE * 128, 9 * D], BF16, kind="Internal")
    wb_up = dram("wb_up", [NE * 128, 9 * D], BF16, kind="Internal")
    wb_down = dram("wb_down", [NE * 128, 9 * D], BF16, kind="Internal")
    rope_all = dram("rope_all", [SEQ, 128], F32)
    rope_own = dram("rope_own", [NOWN, 128], F32)
    dft_c = dram("dft_c", [SEQ, NOWN], BF16)
    dft_s = dram("dft_s", [SEQ, NOWN], BF16)
    cbd = dram("cbd", [256, 512], F32)
    ident_d = dram("ident_d", [128, 128], F32)
    tri_d = dram("tri_d", [128, 128], F32)
    iota_d = dram("iota_d", [128, NE + NSLOT + 1], F32)
    uu_d = dram("uu_d", [NE, 2 * NE], F32)
    out_d = dram("out", [NOWN, D], F32, kind="ExternalOutput")
    xs_d = dram("xs_scr", [NSLOT * 128, D + 32], BF16, kind="Internal")
    gs_d = dram("gs_scr", [NSLOT * 128, 16], F32, kind="Internal")
    ys_d = dram("ys_scr", [NSLOT * 128, D], F32, kind="Internal")
    dbg_out = {}

    S = Sch(nc, es)

    def sb(name, shape, dt):
        return es.enter_context(nc.sbuf_tensor(name, list(shape), dt))

    def add_dbg(name, tile_ap, shape, dt=F32):
        if name in dbg:
            d_ = dram("dbg_" + name, shape, dt, kind="ExternalOutput")
            dbg_out[name] = (d_, tile_ap)

    ident_f = sb("ident_f", [128, 128], F32)
    ident_b = sb("ident_b", [128, 128], BF16)
    ones_b = sb("ones_b", [128, 128], BF16)
    ones_f = sb("ones_f", [128, 128], F32)
    S.dma("sp", "c0", ident_f[:], ident_d, w=["ident_f"])
    S.op("dve", lambda e: e.tensor_copy(out=ident_b[:], in_=ident_f[:]), r=["ident_f"], w=["ident_b"])
    S.op("dve", lambda e: e.memset(ones_b[:], 1.0), w=["ones_b"])
    S.op("dve", lambda e: e.memset(ones_f[:], 1.0), w=["ones_f"])

    epsb = sb("epsb", [128, 1], F32)
    S.op("dve", lambda e: e.memset(epsb[:], EPS), w=["epsb"])

    modn = ["A1", "B1", "cA1", "cB1", "A2", "B2", "G1", "G2"]
    mod = {n: sb("mod_" + n, [128, D], F32) for n in modn}

    def bcast_row(name, src_row, n, q="sp"):
        t = sb(name, [128, n], F32)
        S.dma(q, "c0", t[:], src_row.partition_broadcast(128), w=[name])
        return t

    with ExitStack() as p0:
        def sb0(name, shape, dt):
            return p0.enter_context(nc.sbuf_tensor(name, list(shape), dt))

        def ps0(name, shape, dt):
            return p0.enter_context(nc.psum_tensor(name, list(shape), dt))
        ccol = sb0("ccol", [128, 16], F32)
        S.dma("sp", "c0", ccol[:, 0:8], c_col, w=["ccol"])
        S.dma("sp", "c0", ccol[:, 8:16], cc_col, w=["ccol"])
        scol = sb0("scol", [128, 16], F32)
        S.op("act", lambda e: e.activation(out=scol[:], in_=ccol[:], func=AF.Silu), r=["ccol"], w=["scol"])
        sbc = sb0("sbc", [128, 16, 128], F32)
        for kc in range(16):
            S.op("dve", lambda e, kc=kc: e.tensor_scalar(out=sbc[:, kc, :], in0=ones_f[:], scalar1=scol[:, kc:kc + 1],
                                                      scalar2=None, op0=OP.mult), r=["scol", "ones_f"], w=["sbc"])
        gap = sb0("gap", [128, D], F32)
        gpo = sb0("gpo", [128, D], F32)
        gfp = sb0("gfp", [128, D], F32)
        gfo = sb0("gfo", [128, D], F32)
        for t, src, nm in ((gap, g_attn_pre, "gap"), (gpo, g_attn_post, "gpo"), (gfp, g_ffn_pre, "gfp"), (gfo, g_ffn_post, "gfo")):
            S.dma("sp", "c0", t[:], src.partition_broadcast(128), w=[nm])
        wm = [sb0("wm%d" % i, [128, 8, 512], F32) for i in range(2)]
        bm = [sb0("bm%d" % i, [128, 512], F32) for i in range(2)]
        pm = [ps0("pm%d" % i, [128, 512], F32) for i in range(2)]
        jobs = [(nb, 0) for nb in range(12)] + [(nb, 1) for nb in range(4)]
        for it, (nb, isctx) in enumerate(jobs):
            bi = it % 2
            wk, bk, pk = "wm%d" % bi, "bm%d" % bi, "pm%d" % bi
            S.dma("sp", wk, wm[bi][:], w_mod[:, nb * 512:(nb + 1) * 512].rearrange("(kc p) n -> p kc n", p=128), w=[wk])
            S.dma("sp", wk, bm[bi][:], b_mod[:, nb * 512:(nb + 1) * 512].partition_broadcast(128), w=[bk])
            for kc in range(8):
                S.op("pe", lambda e, kc=kc, bi=bi, isctx=isctx: e.matmul(pm[bi][:], lhsT=sbc[:, isctx * 8 + kc, :], rhs=wm[bi][:, kc, :],
                                                                   start=(kc == 0), stop=(kc == 7)),
                     r=[wk, "sbc"], w=[pk])
            half = slice((nb % 2) * 512, (nb % 2) * 512 + 512)
            grp = nb // 2
            if isctx:
                dst = {0: "cB1", 1: "cA1"}[grp]
            else:
                dst = {0: "B1", 1: "A1", 2: "G1", 3: "B2", 4: "A2", 5: "G2"}[grp]
            gsrc = {"A1": gap, "cA1": gap, "A2": gfp, "G1": gpo, "G2": gfo}.get(dst)
            gnm = {"A1": "gap", "cA1": "gap", "A2": "gfp", "G1": "gpo", "G2": "gfo"}.get(dst)
            dt_ = mod[dst]
            S.op("dve", lambda e, dt_=dt_, bi=bi, half=half: e.tensor_tensor(out=dt_[:, half], in0=pm[bi][:], in1=bm[bi][:], op=OP.add),
                 r=[pk, bk], w=[dst])
            if dst in ("A1", "cA1", "A2"):
                S.op("dve", lambda e, dt_=dt_, half=half, gsrc=gsrc: e.scalar_tensor_tensor(out=dt_[:, half], in0=dt_[:, half], scalar=1.0,
                                                                                        in1=gsrc[:, half], op0=OP.add, op1=OP.mult),
                     r=[dst, gnm], w=[dst])
            elif dst in ("G1", "G2"):
                S.op("dve", lambda e, dt_=dt_, half=half, gsrc=gsrc: e.tensor_tensor(out=dt_[:, half], in0=dt_[:, half], in1=gsrc[:, half], op=OP.mult),
                     r=[dst, gnm], w=[dst])
        S.barrier()
    for n in modn:
        add_dbg(n, mod[n][:], [128, D])


    def flush_dbg():
        for name, (d_, ap_) in list(dbg_out.items()):
            if ap_ is not None:
                S.dma("sp", "dbg", d_, ap_, r=[], w=[])
                dbg_out[name] = (d_, None)
        S.barrier()

    flush_dbg()
    if stage < 1:
        S.barrier(); es.close(); return nc, list(dbg_out)

    x1_d = dram("x1_scr", [NOWN, D], F32, kind="Internal")
    h2_d = dram("h2_scr", [NOWN, D], BF16, kind="Internal")

    scA = ExitStack()
    sc2 = ExitStack()
    sc2q = ExitStack()
    sc3 = ExitStack()

    def close_all():
        sc2q.close(); sc2.close(); sc3.close(); scA.close()

    def sbA(name, shape, dt):
        return scA.enter_context(nc.sbuf_tensor(name, list(shape), dt))
    kvnT = sbA("kvnT", [128, 2, NKEY], BF16)
    kropeT = sbA("kropeT", [128, NKEY], BF16)
    fourT = sbA("fourT", [128, 2, NOWN], BF16)

    def rstd(stt, ci, co, key):
        S.op("act", lambda e: e.activation(out=stt[:, co:co + 1], in_=stt[:, ci:ci + 1], func=AF.Sqrt, bias=epsb[:, 0:1], scale=1.0), r=[key, "epsb"], w=[key])
        S.op("dve", lambda e: e.reciprocal(out=stt[:, co:co + 1], in_=stt[:, co:co + 1]), r=[key], w=[key])

    def hx_pipeline(sbx, ti, src_ap, Akey, Bkey, xin, st, tmpf, hxb, tp_hx, hxT, junk):
        i3, i2 = ti % 3, ti % 2
        S.op("act", lambda e: e.activation(out=junk[:], in_=xin[i3][:], func=AF.Square, scale=1.0 / 32.0, accum_out=st[i2][:, 0:1]),
             r=["xin%d" % i3], w=["junk", "st%d" % i2])
        rstd(st[i2], 0, 1, "st%d" % i2)
        S.op("dve", lambda e: e.scalar_tensor_tensor(out=tmpf[i2][:], in0=xin[i3][:], scalar=st[i2][:, 1:2], in1=mod[Akey][:],
                                                     op0=OP.mult, op1=OP.mult),
             r=["xin%d" % i3, "st%d" % i2], w=["tmpf%d" % i2])
        S.op("dve", lambda e: e.tensor_tensor(out=hxb[i2][:], in0=tmpf[i2][:], in1=mod[Bkey][:], op=OP.add),
             r=["tmpf%d" % i2], w=["hxb%d" % i2])
        for kc in range(8):
            S.op("pe", lambda e, kc=kc: e.transpose(out=tp_hx[i2][:, kc, :], in_=hxb[i2][:, kc * 128:(kc + 1) * 128], identity=ident_b[:]),
                 r=["hxb%d" % i2], w=["tp_hx%d" % i2])
        S.op("act", lambda e: e.activation(out=hxT[i2][:], in_=tp_hx[i2][:], func=AF.Copy), r=["tp_hx%d" % i2], w=["hxT%d" % i2])

    sc1 = ExitStack()

    def sb1(name, shape, dt):
        return sc1.enter_context(nc.sbuf_tensor(name, list(shape), dt))

    def ps1(name, shape, dt):
        return sc1.enter_context(nc.psum_tensor(name, list(shape), dt))
    PQ = sb1("PQ", [128, 64, 512], BF16)
    w1 = sb1("w1", [128, 8, 832], BF16)
    gkv = sb1("gkv", [128, 256], F32)
    S.dma("sp", "c0", gkv[:], g_kv.partition_broadcast(128), w=["gkv"])
    S.dma("pool", "w1a", w1[:, :, 0:320], w_in[:, 640:960].rearrange("(kc p) n -> p kc n", p=128), w=["w1"])
    with ExitStack() as pw:
        def sbw(name, shape, dt):
            return pw.enter_context(nc.sbuf_tensor(name, list(shape), dt))

        def psw(name, shape, dt):
            return pw.enter_context(nc.psum_tensor(name, list(shape), dt))
        cbd_sb = sbw("cbd_sb", [128, 2, 512], F32)
        wf_sb = sbw("wf_sb", [128, 2, 64], F32)
        mbd = sbw("mbd", [128, 2, 512], BF16)
        wu = sbw("wu", [128, 8, 256], BF16)
        wuT = sbw("wuT", [128, 2, D], BF16)
        S.dma("sp", "c0", cbd_sb[:], cbd.rearrange("(kc p) n -> p kc n", p=128), w=["cbd_sb"])
        S.dma("sp", "c0", wf_sb[:], w_four.rearrange("(kc p) n -> p kc n", p=128), w=["wf_sb"])
        S.dma("pool", "w1a", wu[:], w_in[:, 0:256].rearrange("(kc p) n -> p kc n", p=128), w=["wu"])
        S.op("dve", lambda e: e.memset(mbd[:], 0.0), w=["mbd"])
        pcw = psw("pcw", [128, 512], F32)
        for mc in range(2):
            for tr in range(2):
                o = (mc * 2 + tr) * 64
                S.op("pe", lambda e, mc=mc, tr=tr, o=o: e.matmul(pcw[:, o:o + 64], lhsT=cbd_sb[:, mc, tr * 256 + mc * 128: tr * 256 + mc * 128 + 128],
                                                              rhs=wf_sb[:, mc, :], start=True, stop=True),
                     r=["cbd_sb", "wf_sb"], w=["pcw"])
        for mc in range(2):
            for tr in range(2):
                o = (mc * 2 + tr) * 64
                for gl in range(2):
                    g = 2 * mc + gl
                    S.op("dve", lambda e, mc=mc, tr=tr, o=o, gl=gl, g=g: e.tensor_copy(
                        out=mbd[gl * 64:(gl + 1) * 64, mc, tr * 256 + g * 64: tr * 256 + (g + 1) * 64],
                        in_=pcw[gl * 64:(gl + 1) * 64, o:o + 64]), r=["pcw"], w=["mbd"])
        ptw = [psw("ptw%d" % i, [128, 1024], BF16) for i in range(2)]
        for kc in range(2):
            for dc in range(8):
                S.op("pe", lambda e, kc=kc, dc=dc: e.transpose(out=ptw[kc][:, dc * 128:(dc + 1) * 128], in_=wu[:, dc, kc * 128:(kc + 1) * 128],
                                                            identity=ident_b[:]), r=["wu", "ident_b"], w=["ptw%d" % kc])
            S.op("act", lambda e, kc=kc: e.activation(out=wuT[:, kc, :], in_=ptw[kc][:], func=AF.Copy), r=["ptw%d" % kc], w=["wuT"])
        ppq = [psw("ppq%d" % i, [128, 512], F32) for i in range(2)]
        for dc in range(8):
            for kc in range(2):
                S.op("pe", lambda e, kc=kc, dc=dc: e.matmul(ppq[dc % 2][:], lhsT=wuT[:, kc, dc * 128:(dc + 1) * 128], rhs=mbd[:, kc, :],
                                                         start=(kc == 0), stop=(kc == 1)), r=["wuT", "mbd"], w=["ppq%d" % (dc % 2)])
            S.op("dve", lambda e, dc=dc: e.tensor_copy(out=w1[:, dc, 320:832], in_=ppq[dc % 2][:]), r=["ppq%d" % (dc % 2)], w=["w1"])
        S.barrier()

    sc1t = ExitStack()

    def sb1(name, shape, dt):
        return sc1t.enter_context(nc.sbuf_tensor(name, list(shape), dt))

    def ps1(name, shape, dt):
        return sc1t.enter_context(nc.psum_tensor(name, list(shape), dt))
    xin = [sb1("xin%d" % i, [128, D], F32) for i in range(3)]
    st = [sb1("st%d" % i, [128, 8], F32) for i in range(2)]
    tmpf = [sb1("tmpf%d" % i, [128, D], F32) for i in range(2)]
    hxb = [sb1("hxb%d" % i, [128, D], BF16) for i in range(2)]
    hxT = [sb1("hxT%d" % i, [128, 8, 128], BF16) for i in range(2)]
    junk = sb1("junk", [128, D], F32)
    rp = [sb1("rp%d" % i, [128, 128], F32) for i in range(2)]
    kraw = [sb1("kraw%d" % i, [128, 64], F32) for i in range(2)]
    rt = [sb1("rt%d" % i, [128, 128], F32) for i in range(2)]
    kvr = [sb1("kvr%d" % i, [128, 384], BF16) for i in range(2)]
    tp_hx = [ps1("tp_hx%d" % i, [128, 8, 128], BF16) for i in range(2)]
    ps_kv = [ps1("ps_kv%d" % i, [128, 512], F32) for i in range(2)]
    ps_pq = [ps1("ps_pq%d" % i, [128, 512], F32) for i in range(2)]
    tp_kv = ps1("tp_kv", [128, 8, 128], BF16)

    n_t1 = NKT if stage >= 2 else 4
    for ti in range(n_t1):
        isctx = ti >= 64
        i2 = ti % 2
        def stage_a(t_):
            c_ = t_ >= 64
            src_ = ctx_b[(t_ - 64) * 128:(t_ - 63) * 128, :] if c_ else x_b[t_ * 128:(t_ + 1) * 128, :]
            hx_pipeline(sb1, t_, src_, "cA1" if c_ else "A1", "cB1" if c_ else "B1", xin, st, tmpf, hxb, tp_hx, hxT, junk)
        def load_x(t_):
            c_ = t_ >= 64
            src_ = ctx_b[(t_ - 64) * 128:(t_ - 63) * 128, :] if c_ else x_b[t_ * 128:(t_ + 1) * 128, :]
            S.dma("sp", "xin%d" % (t_ % 3), xin[t_ % 3][:], src_, w=["xin%d" % (t_ % 3)])
        if ti == 0:
            load_x(0)
            if n_t1 > 1:
                load_x(1)
            stage_a(0)
        if ti + 2 < n_t1:
            load_x(ti + 2)
        if ti + 1 < n_t1:
            stage_a(ti + 1)
        if ti == 0:
            add_dbg("hxb0", hxb[0][:], [128, D], BF16)
            flush_dbg()
        if not isctx:
            S.dma("sp", "rp%d" % i2, rp[i2][:], rope_all[ti * 128:(ti + 1) * 128, :], w=["rp%d" % i2])
        for kc in range(8):
            S.op("pe", lambda e, kc=kc: e.matmul(ps_kv[i2][:, 0:320], lhsT=hxT[i2][:, kc, :], rhs=w1[:, kc, 0:320], start=(kc == 0), stop=(kc == 7)),
                 r=["hxT%d" % i2, "w1"], w=["ps_kv%d" % i2])
        if not isctx:
            for kc in range(8):
                S.op("pe", lambda e, kc=kc: e.matmul(ps_pq[i2][:], lhsT=hxT[i2][:, kc, :], rhs=w1[:, kc, 320:832], start=(kc == 0), stop=(kc == 7)),
                     r=["hxT%d" % i2, "w1"], w=["ps_pq%d" % i2])
        S.op("act", lambda e: e.activation(out=junk[:, 0:256], in_=ps_kv[i2][:, 0:256], func=AF.Square, scale=1.0 / 16.0, accum_out=st[i2][:, 2:3]),
             r=["ps_kv%d" % i2], w=["junk", "st%d" % i2])
        rstd(st[i2], 2, 3, "st%d" % i2)
        S.op("dve", lambda e: e.scalar_tensor_tensor(out=kvr[i2][:, 0:256], in0=ps_kv[i2][:, 0:256], scalar=st[i2][:, 3:4], in1=gkv[:],
                                                     op0=OP.mult, op1=OP.mult), r=["ps_kv%d" % i2, "st%d" % i2, "gkv"], w=["kvr%d" % i2])
        if isctx:
            for o_ in (256, 320):
                S.op("act", lambda e, o_=o_: e.activation(out=kvr[i2][:, o_:o_ + 64], in_=ps_kv[i2][:, 256:320], func=AF.Copy),
                     r=["ps_kv%d" % i2], w=["kvr%d" % i2])
        else:
            S.op("act", lambda e: e.activation(out=kraw[i2][:], in_=ps_kv[i2][:, 256:320], func=AF.Copy), r=["ps_kv%d" % i2], w=["kraw%d" % i2])
            S.op("pool", lambda e: e.tensor_tensor(out=rt[i2][:, 0:64], in0=kraw[i2][:], in1=rp[i2][:, 0:64], op=OP.mult),
                 r=["kraw%d" % i2, "rp%d" % i2], w=["rt%d" % i2])
            S.op("pool", lambda e: e.tensor_tensor(out=rt[i2][:, 64:128:2], in0=kraw[i2][:, 1:64:2], in1=rp[i2][:, 64:128:2], op=OP.mult),
                 r=["kraw%d" % i2, "rp%d" % i2], w=["rt%d" % i2])
            S.op("pool", lambda e: e.tensor_tensor(out=rt[i2][:, 65:128:2], in0=kraw[i2][:, 0:64:2], in1=rp[i2][:, 65:128:2], op=OP.mult),
                 r=["kraw%d" % i2, "rp%d" % i2], w=["rt%d" % i2])
            for o_ in (256, 320):
                S.op("pool", lambda e, o_=o_: e.tensor_tensor(out=kvr[i2][:, o_:o_ + 64], in0=rt[i2][:, 0:64], in1=rt[i2][:, 64:128], op=OP.add),
                     r=["rt%d" % i2], w=["kvr%d" % i2])
            S.op("act", lambda e: e.activation(out=PQ[:, ti, :], in_=ps_pq[i2][:], func=AF.Copy), r=["ps_pq%d" % i2], w=[("PQ", ti)])
        for c3 in range(3):
            S.op("pe", lambda e, c3=c3: e.transpose(out=tp_kv[:, c3, :], in_=kvr[i2][:, c3 * 128:c3 * 128 + 128], identity=ident_b[:]),
                 r=["kvr%d" % i2], w=["tp_kv"])
        S.op("dve", lambda e: e.tensor_copy(out=kvnT[:, :, ti * 128:(ti + 1) * 128], in_=tp_kv[:, 0:2, :]), r=["tp_kv"], w=[("kvnT", ti)])
        S.op("dve", lambda e: e.tensor_copy(out=kropeT[:, ti * 128:(ti + 1) * 128], in_=tp_kv[:, 2, :]), r=["tp_kv"], w=[("kropeT", ti)])
    S.barrier()
    add_dbg("kvnT", kvnT[:, :, 0:512], [128, 2, 512], BF16)
    add_dbg("kropeT", kropeT[:, 0:512], [128, 512], BF16)
    add_dbg("PQ0", PQ[:, 0, :], [128, 512], BF16)
    flush_dbg()
    sc1t.close()
    if stage < 3:
        sc1.close(); close_all(); S.barrier(); es.close(); return nc, list(dbg_out)

    with ExitStack() as pf_:
        tabc = [pf_.enter_context(nc.sbuf_tensor("tabc%d" % i, [128, NOWN], BF16)) for i in range(4)]
        tabs = [pf_.enter_context(nc.sbuf_tensor("tabs%d" % i, [128, NOWN], BF16)) for i in range(4)]
        pf = [[pf_.enter_context(nc.psum_tensor("pf%d_%d" % (c, kb), [128, 512], F32)) for kb in range(4)] for c in range(2)]
        for t in range(64):
            i2 = t % 4
            S.dma("sp", "tabc%d" % i2, tabc[i2][:], dft_c[t * 128:(t + 1) * 128, :], w=["tabc%d" % i2])
            S.dma("pool", "tabs%d" % i2, tabs[i2][:], dft_s[t * 128:(t + 1) * 128, :], w=["tabs%d" % i2])
            for c in range(2):
                for (tab, tn, off) in ((tabc, "tabc", 0), (tabs, "tabs", 256)):
                    for kb in range(4):
                        S.op("pe", lambda e, c=c, tab=tab, off=off, kb=kb: e.matmul(
                            pf[c][kb][:], lhsT=PQ[:, t, off + c * 128: off + c * 128 + 128], rhs=tab[i2][:, kb * 512:(kb + 1) * 512],
                            start=(t == 0 and off == 0), stop=(t == 63 and off == 256)),
                            r=["%s%d" % (tn, i2)], w=["pf%d_%d" % (c, kb)])
        for c in range(2):
            for kb in range(4):
                eng = "act" if kb % 2 == 0 else "dve"
                if eng == "act":
                    S.op("act", lambda e, c=c, kb=kb: e.activation(out=fourT[:, c, kb * 512:(kb + 1) * 512], in_=pf[c][kb][:], func=AF.Copy),
                         r=["pf%d_%d" % (c, kb)], w=["fourT"])
                else:
                    S.op("dve", lambda e, c=c, kb=kb: e.tensor_copy(out=fourT[:, c, kb * 512:(kb + 1) * 512], in_=pf[c][kb][:]),
                         r=["pf%d_%d" % (c, kb)], w=["fourT"])
        S.barrier()
    add_dbg("fourT", fourT[:], [128, 2, NOWN], BF16)
    flush_dbg()
    sc1.close()
    if stage < 4:
        close_all(); S.barrier(); es.close(); return nc, list(dbg_out)


    def sb2q(name, shape, dt):
        return sc2q.enter_context(nc.sbuf_tensor(name, list(shape), dt))
    q_nopeT = sb2q("q_nopeT", [128, NH, NOWN], BF16)
    q_ropeT = sb2q("q_ropeT", [128, 3, NOWN], BF16)
    wkv = sb2q("wkv", [128, 2, 1536], BF16)
    S.dma("pool", "wl", wkv[:], w_kv_up.rearrange("(kc p) n -> p kc n", p=128), w=["wkv"])
    sc2t = ExitStack()

    def sb2(name, shape, dt):
        return sc2t.enter_context(nc.sbuf_tensor("b_" + name, list(shape), dt, side="right"))

    def ps2(name, shape, dt):
        return sc2t.enter_context(nc.psum_tensor("b_" + name, list(shape), dt))
    w2 = sb2("w2", [128, 8, 384], BF16)
    wq = sb2("wq", [128, 3, 1152], BF16)
    gq = sb2("gq", [128, 384], F32)
    S.dma("pool", "wl", w2[:], w_in[:, 256:640].rearrange("(kc p) n -> p kc n", p=128), w=["w2"])
    S.dma("pool", "wl", wq[:], w_q_up.rearrange("(kc p) n -> p kc n", p=128), w=["wq"])
    S.dma("sp", "c0", gq[:], g_q.partition_broadcast(128), w=["gq"])
    xin = [sb2("xin%d" % i, [128, D], F32) for i in range(3)]
    st = [sb2("st%d" % i, [128, 8], F32) for i in range(2)]
    tmpf = [sb2("tmpf%d" % i, [128, D], F32) for i in range(2)]
    hxb = [sb2("hxb%d" % i, [128, D], BF16) for i in range(2)]
    hxT = [sb2("hxT%d" % i, [128, 8, 128], BF16) for i in range(2)]
    junk = sb2("junk", [128, D], F32)
    rp = [sb2("rp%d" % i, [128, 128], F32) for i in range(2)]
    qnb = [sb2("qnb%d" % i, [128, 384], BF16) for i in range(2)]
    qnT = [sb2("qnT%d" % i, [128, 3, 128], BF16) for i in range(2)]
    qf = [sb2("qf%d" % i, [128, NH, 192], F32) for i in range(2)]
    qbn = [sb2("qbn%d" % i, [128, NH, 128], BF16) for i in range(2)]
    rq = [sb2("rq%d" % i, [128, NH, 128], F32) for i in range(2)]
    qrp = [sb2("qrp%d" % i, [128, NH, 64], BF16) for i in range(2)]
    tp_hx = [ps2("tp_hx%d" % i, [128, 8, 128], BF16) for i in range(2)]
    ps_q1 = ps2("ps_q1", [128, 512], F32)
    tp_qr = ps2("tp_qr", [128, 8, 128], BF16)
    tp_q = tp_qr[:, 0:3, :]
    ps_q = [ps2("ps_qq%d" % i, [128, 512], F32) for i in range(3)]
    tpn = ps2("tpn", [128, 8, 128], BF16)
    tpr = tp_qr[:, 3:6, :]

    def hx_keys_fix(i2):
        return "tp_hx0"
    n_t1b = NTO if stage >= 5 else 2
    for ti in range(n_t1b):
        i2 = ti % 2
        def load_xo(t_):
            S.dma("sp", "xin%d" % (t_ % 3), xin[t_ % 3][:], x_own[t_ * 128:(t_ + 1) * 128, :], w=["xin%d" % (t_ % 3)])
        if ti == 0:
            load_xo(0)
            if n_t1b > 1:
                load_xo(1)
            hx_pipeline(sb2, 0, x_own[0:128, :], "A1", "B1", xin, st, tmpf, hxb, tp_hx, hxT, junk)
        if ti + 2 < n_t1b:
            load_xo(ti + 2)
        if ti + 1 < n_t1b:
            hx_pipeline(sb2, ti + 1, x_own[(ti + 1) * 128:(ti + 2) * 128, :], "A1", "B1", xin, st, tmpf, hxb, tp_hx, hxT, junk)
        S.dma("sp", "rp%d" % i2, rp[i2][:], rope_own[ti * 128:(ti + 1) * 128, :], w=["rp%d" % i2])
        for kc in range(8):
            S.op("pe", lambda e, kc=kc: e.matmul(ps_q1[:, 0:384], lhsT=hxT[i2][:, kc, :], rhs=w2[:, kc, :], start=(kc == 0), stop=(kc == 7)),
                 r=["hxT%d" % i2, "w2"], w=["ps_q1"])
        S.op("act", lambda e: e.activation(out=junk[:, 0:384], in_=ps_q1[:, 0:384], func=AF.Square, scale=float(384.0 ** -0.5), accum_out=st[i2][:, 2:3]),
             r=["ps_q1"], w=["junk", "st%d" % i2])
        rstd(st[i2], 2, 3, "st%d" % i2)
        S.op("dve", lambda e: e.scalar_tensor_tensor(out=qnb[i2][:], in0=ps_q1[:, 0:384], scalar=st[i2][:, 3:4], in1=gq[:], op0=OP.mult, op1=OP.mult),
             r=["ps_q1", "st%d" % i2, "gq"], w=["qnb%d" % i2])
        for c3 in range(3):
            S.op("pe", lambda e, c3=c3: e.transpose(out=tp_q[:, c3, :], in_=qnb[i2][:, c3 * 128:(c3 + 1) * 128], identity=ident_b[:]),
                 r=["qnb%d" % i2], w=["tp_q"])
        S.op("act", lambda e: e.activation(out=qnT[i2][:], in_=tp_q, func=AF.Copy), r=["tp_q"], w=["qnT%d" % i2])
        qf_flat = qf[i2][:].rearrange("p h d -> p (h d)")
        for nb, (c0, c1) in enumerate(((0, 512), (512, 1024), (1024, 1152))):
            for kc in range(3):
                S.op("pe", lambda e, kc=kc, nb=nb, c0=c0, c1=c1: e.matmul(ps_q[nb][:, 0:c1 - c0], lhsT=qnT[i2][:, kc, :], rhs=wq[:, kc, c0:c1],
                                                                      start=(kc == 0), stop=(kc == 2)), r=["qnT%d" % i2, "wq"], w=["ps_q%d" % nb])
            S.op("act", lambda e, nb=nb, c0=c0, c1=c1: e.activation(out=qf_flat[:, c0:c1], in_=ps_q[nb][:, 0:c1 - c0], func=AF.Copy),
                 r=["ps_q%d" % nb], w=["qf%d" % i2])
        S.op("dve", lambda e: e.tensor_copy(out=qbn[i2][:], in_=qf[i2][:, :, 0:128]), r=["qf%d" % i2], w=["qbn%d" % i2])
        cosb = rp[i2][:, 0:64].unsqueeze(1).to_broadcast([128, NH, 64])
        sineb = rp[i2][:, 64:128:2].unsqueeze(1).to_broadcast([128, NH, 32])
        sinob = rp[i2][:, 65:128:2].unsqueeze(1).to_broadcast([128, NH, 32])
        S.op("pool", lambda e: e.tensor_tensor(out=rq[i2][:, :, 0:64], in0=qf[i2][:, :, 128:192], in1=cosb, op=OP.mult),
             r=["qf%d" % i2, "rp%d" % i2], w=["rq%d" % i2])
        S.op("pool", lambda e: e.tensor_tensor(out=rq[i2][:, :, 64:128:2], in0=qf[i2][:, :, 129:192:2], in1=sineb, op=OP.mult),
             r=["qf%d" % i2, "rp%d" % i2], w=["rq%d" % i2])
        S.op("pool", lambda e: e.tensor_tensor(out=rq[i2][:, :, 65:128:2], in0=qf[i2][:, :, 128:192:2], in1=sinob, op=OP.mult),
             r=["qf%d" % i2, "rp%d" % i2], w=["rq%d" % i2])
        S.op("pool", lambda e: e.tensor_tensor(out=qrp[i2][:], in0=rq[i2][:, :, 0:64], in1=rq[i2][:, :, 64:128], op=OP.add),
             r=["rq%d" % i2], w=["qrp%d" % i2])
        for h in range(NH):
            S.op("pe", lambda e, h=h: e.transpose(out=tpn[:, h, :], in_=qbn[i2][:, h, :], identity=ident_b[:]), r=["qbn%d" % i2], w=["tpn"])
        qrp2 = qrp[i2][:].rearrange("p (s a) d -> p s (a d)", a=2)
        for s_ in range(3):
            S.op("pe", lambda e, s_=s_: e.transpose(out=tpr[:, s_, :], in_=qrp2[:, s_, :], identity=ident_b[:]), r=["qrp%d" % i2], w=["tpr"])
        S.op("act", lambda e: e.activation(out=q_nopeT[:, :, ti * 128:(ti + 1) * 128], in_=tpn[:, 0:NH, :], func=AF.Copy), r=["tpn"], w=[("qn", ti)])
        S.op("dve", lambda e: e.tensor_copy(out=q_ropeT[:, :, ti * 128:(ti + 1) * 128], in_=tpr), r=["tpr"], w=[("qr", ti)])
    S.barrier()
    add_dbg("q_nopeT", q_nopeT[:, :, 0:256], [128, NH, 256], BF16)
    add_dbg("q_ropeT", q_ropeT[:, :, 0:256], [128, 3, 256], BF16)
    flush_dbg()
    sc2t.close()
    def sb3(name, shape, dt):
        return sc3.enter_context(nc.sbuf_tensor(name, list(shape), dt, side="right"))
    lg = sb3("lg", [128, NTO, NE], F32)
    d4i = sb3("d4i", [128, NTO * 4], I32)
    beci = sb3("beci", [128, 2 * NSLOT], I32)
    idxw = sb3("idxw", [128, NSLOT], I32)
    attT = sc2.enter_context(nc.sbuf_tensor("attT", [128, NH, NOWN], BF16, side="right"))
    if stage < 5:
        close_all(); S.barrier(); es.close(); return nc, list(dbg_out)

    if stage >= 8:
        ncast = 0
        for ex in range(NE):
            for (wsrc, wdst) in ((wa_gate, wb_gate), (wa_up, wb_up), (wa_down, wb_down)):
                S.dma("pool", "wc%d" % (ncast % 4), wdst[ex * 128:(ex + 1) * 128, :], wsrc[ex * 128:(ex + 1) * 128, :], w=[("wb", ncast)])
                ncast += 1
    with ExitStack() as pa:
        G = 3
        NG = NKT // G
        KnT = pa.enter_context(nc.sbuf_tensor("KnT", [128, NKEY], BF16))
        Vh = pa.enter_context(nc.sbuf_tensor("Vh", [128, NKT, 128], BF16))
        PT = [pa.enter_context(nc.sbuf_tensor("PT%d" % i, [128, G * 512], BF16)) for i in range(3)]
        dacc = [pa.enter_context(nc.sbuf_tensor("dacc%d" % i, [128, 512], F32)) for i in range(2)]
        ps_s = [pa.enter_context(nc.psum_tensor("ps_s%d" % i, [128, G * 512], F32)) for i in range(2)]
        ps_o = pa.enter_context(nc.psum_tensor("ps_o", [128, 512], F32))
        ps_d = pa.enter_context(nc.psum_tensor("ps_d", [128, 512], F32))
        n_heads = NH if stage >= 6 else 1
        n_qb = 4 if stage >= 6 else 1
        cnt_ev = 0
        for h in range(n_heads):
            S._wait("pe", "act", S.cnt["act"])
            S._wait("pe", "dve", S.cnt["dve"])
            nbk = 0
            for cb in range(17):
                n = 512 if cb < 16 else NKEY - 16 * 512
                bkey = "psb%d" % (nbk % 6)
                pk = ps_s[(nbk % 6) // 3][:, ((nbk % 6) % 3) * 512:((nbk % 6) % 3 + 1) * 512]
                nbk += 1
                for kc in range(2):
                    S.op("pe", lambda e, kc=kc, cb=cb, n=n, pk=pk: e.matmul(pk[:, 0:n], lhsT=wkv[:, kc, h * 256:h * 256 + 128],
                                                                    rhs=kvnT[:, kc, cb * 512:cb * 512 + n], start=(kc == 0), stop=(kc == 1)),
                         r=["wkv"], w=[bkey])
                if cnt_ev % 2 == 0:
                    S.op("act", lambda e, cb=cb, n=n, pk=pk: e.activation(out=KnT[:, cb * 512:cb * 512 + n], in_=pk[:, 0:n], func=AF.Copy),
                         r=[bkey], w=[("KnT", cb)])
                else:
                    S.op("dve", lambda e, cb=cb, n=n, pk=pk: e.tensor_copy(out=KnT[:, cb * 512:cb * 512 + n], in_=pk[:, 0:n]),
                         r=[bkey], w=[("KnT", cb)])
                cnt_ev += 1
            for g4 in range(17):
                kts = list(range(4 * g4, min(4 * g4 + 4, NKT)))
                bkey = "psb%d" % (nbk % 6)
                pk = ps_s[(nbk % 6) // 3][:, ((nbk % 6) % 3) * 512:((nbk % 6) % 3 + 1) * 512]
                nbk += 1
                for i_, kt in enumerate(kts):
                    for kc in range(2):
                        S.op("pe", lambda e, kc=kc, kt=kt, i_=i_, pk=pk: e.matmul(pk[:, i_ * 128:(i_ + 1) * 128], lhsT=kvnT[:, kc, kt * 128:(kt + 1) * 128],
                                                                          rhs=wkv[:, kc, h * 256 + 128:h * 256 + 256], start=(kc == 0), stop=(kc == 1)),
                             r=["wkv"], w=[bkey])
                nn = len(kts)
                vdst = Vh[:, kts[0]:kts[0] + nn, :].rearrange("p a d -> p (a d)")
                if cnt_ev % 2 == 0:
                    S.op("act", lambda e, pk=pk, nn=nn, vdst=vdst: e.activation(out=vdst, in_=pk[:, 0:nn * 128], func=AF.Copy),
                         r=[bkey], w=[("Vh", g4)])
                else:
                    S.op("dve", lambda e, pk=pk, nn=nn, vdst=vdst: e.tensor_copy(out=vdst, in_=pk[:, 0:nn * 128]),
                         r=[bkey], w=[("Vh", g4)])
                cnt_ev += 1
            S._wait("pe", "act", S.cnt["act"])
            S._wait("pe", "dve", S.cnt["dve"])
            hp = (h % 2) * 64
            for qb in range(n_qb):
                qs = slice(qb * 512, (qb + 1) * 512)

                def s_mm(g):
                    for i in range(G):
                        kt = g * G + i
                        ks = slice(kt * 128, (kt + 1) * 128)
                        dst = ps_s[g % 2][:, i * 512:(i + 1) * 512]
                        S.op("pe", lambda e: e.matmul(dst, lhsT=KnT[:, ks], rhs=q_nopeT[:, h, qs], start=True, stop=False),
                             r=["KnT"], w=["ps_s%d" % (g % 2)])
                    for i in range(G):
                        kt = g * G + i
                        ks = slice(kt * 128, (kt + 1) * 128)
                        dst = ps_s[g % 2][:, i * 512:(i + 1) * 512]
                        S.op("pe", lambda e: e.matmul(dst, lhsT=kropeT[hp:hp + 64, ks], rhs=q_ropeT[hp:hp + 64, h // 2, qs], start=False, stop=True),
                             r=[], w=["ps_s%d" % (g % 2)])
                s_mm(0)
                used = [False, False]
                for g in range(NG):
                    if g + 1 < NG:
                        s_mm(g + 1)
                    pk_ = "PT%d" % (g % 3)
                    S.op("act", lambda e: e.activation(out=PT[g % 3][:], in_=ps_s[g % 2][:], func=AF.Exp, scale=QSCALE),
                         r=["ps_s%d" % (g % 2)], w=[pk_])
                    for i in range(G):
                        kt = g * G + i
                        S.op("pe", lambda e: e.matmul(ps_o[:], lhsT=Vh[:, kt, :], rhs=PT[g % 3][:, i * 512:(i + 1) * 512],
                                                      start=(kt == 0), stop=(kt == NKT - 1)), r=["Vh", pk_], w=["ps_o"])
                    a_ = 0
                    en_ = "dve"
                    for i in range(G):
                        pti = PT[g % 3][:, i * 512:(i + 1) * 512]
                        if not used[a_]:
                            S.op(en_, lambda e: e.tensor_copy(out=dacc[a_][:], in_=pti), r=[pk_], w=["dacc%d" % a_])
                            used[a_] = True
                        else:
                            S.op(en_, lambda e: e.tensor_tensor(out=dacc[a_][:], in0=dacc[a_][:], in1=pti, op=OP.add),
                                 r=[pk_, "dacc%d" % a_], w=["dacc%d" % a_])
                S.op("pe", lambda e: e.matmul(ps_d[:], lhsT=ones_f[:], rhs=dacc[0][:], start=True, stop=True),
                     r=["dacc0", "ones_f"], w=["ps_d"])
                S.op("dve", lambda e: e.reciprocal(out=dacc[0][:], in_=ps_d[:]), r=["ps_d"], w=["dacc0"])
                S.op("dve", lambda e: e.tensor_tensor(out=attT[:, h, qs], in0=ps_o[:], in1=dacc[0][:], op=OP.mult), r=["ps_o", "dacc0"], w=[("attT", h, qb)])
        S.barrier()
    add_dbg("attT", attT[:], [128, NH, NOWN], BF16)
    flush_dbg()
    sc2q.close()
    if stage < 7:
        close_all(); S.barrier(); es.close(); return nc, list(dbg_out)


    with ExitStack() as p3:
        def sbp(name, shape, dt):
            return p3.enter_context(nc.sbuf_tensor("c_" + name, list(shape), dt))

        def psp(name, shape, dt):
            return p3.enter_context(nc.psum_tensor("c_" + name, list(shape), dt))
        wo = sbp("wo", [128, 8, D], BF16)
        wr = sbp("wr", [128, 8, NE], F32)
        brt = sbp("brt", [128, NE], F32)
        S.dma("pool", "wl", wo[:], w_out.rearrange("(kc p) n -> p kc n", p=128), w=["wo"])
        S.dma("sp", "c0", wr[:], w_router.rearrange("(kc p) n -> p kc n", p=128), w=["wr"])
        S.dma("sp", "c0", brt[:], b_router.partition_broadcast(128), w=["brt"])
        xo = [sbp("xo%d" % i, [128, D], F32) for i in range(2)]
        mixs = [sbp("mixs%d" % i, [128, D], F32) for i in range(2)]
        tmp3 = [sbp("tmp3%d" % i, [128, D], F32) for i in range(2)]
        hx2T = [sbp("hx2T%d" % i, [128, 8, 128], F32) for i in range(2)]
        st3 = [sbp("st3%d" % i, [128, 8], F32) for i in range(2)]
        junk3 = sbp("junk3", [128, D], BF16)
        hx2bt = [sbp("hx2bt%d" % i, [128, D], BF16) for i in range(2)]
        ps_m = [[psp("ps_m%d_%d" % (i, nb), [128, 512], F32) for nb in range(2)] for i in range(2)]
        tp_f = psp("tp_f", [128, D], F32)
        ps_l = psp("ps_l", [128, 512], F32)
        for ti in range(NTO):
            i2 = ti % 2
            cs = slice(ti * 128, (ti + 1) * 128)
            xk, mk, tk, sk = "xo%d" % i2, "mixs%d" % i2, "tmp3%d" % i2, "st3%d" % i2
            if ti == 0:
                S.dma("sp", xk, xo[i2][:], x_own[cs, :], w=[xk])
            if ti + 1 < NTO:
                S.dma("sp", "xo%d" % ((ti + 1) % 2), xo[(ti + 1) % 2][:], x_own[(ti + 1) * 128:(ti + 2) * 128, :], w=["xo%d" % ((ti + 1) % 2)])
            for nb in range(2):
                for c in range(8):
                    lt = attT[:, c, cs] if c < NH else fourT[:, c - NH, cs]
                    S.op("pe", lambda e, nb=nb, c=c, lt=lt: e.matmul(ps_m[i2][nb][:], lhsT=lt, rhs=wo[:, c, nb * 512:(nb + 1) * 512],
                                                                 start=(c == 0), stop=(c == 7)), r=["wo"], w=["ps_m%d_%d" % (i2, nb)])
                S.op("act", lambda e, nb=nb: e.activation(out=mixs[i2][:, nb * 512:(nb + 1) * 512], in_=ps_m[i2][nb][:], func=AF.Copy),
                     r=["ps_m%d_%d" % (i2, nb)], w=[mk])
            S.op("act", lambda e: e.activation(out=junk3[:], in_=mixs[i2][:], func=AF.Square, scale=1.0 / 32.0, accum_out=st3[i2][:, 0:1]),
                 r=[mk], w=["junk3", sk])
            rstd(st3[i2], 0, 1, sk)
            S.op("dve", lambda e: e.scalar_tensor_tensor(out=tmp3[i2][:], in0=mixs[i2][:], scalar=st3[i2][:, 1:2], in1=mod["G1"][:], op0=OP.mult, op1=OP.mult),
                 r=[mk, sk], w=[tk])
            S.op("pool", lambda e: e.tensor_tensor(out=xo[i2][:], in0=tmp3[i2][:], in1=xo[i2][:], op=OP.add), r=[tk, xk], w=[xk])
            S.dma("sp", "x1st", x1_d[cs, :], xo[i2][:], r=[xk], w=[("x1d", ti)])
            S.op("act", lambda e: e.activation(out=junk3[:], in_=xo[i2][:], func=AF.Square, scale=1.0 / 32.0, accum_out=st3[i2][:, 2:3]),
                 r=[xk], w=["junk3", sk])
            rstd(st3[i2], 2, 3, sk)
            S.op("dve", lambda e: e.scalar_tensor_tensor(out=tmp3[i2][:], in0=xo[i2][:], scalar=st3[i2][:, 3:4], in1=mod["A2"][:], op0=OP.mult, op1=OP.mult),
                 r=[xk, sk], w=[tk])
            S.op("pool", lambda e: e.tensor_tensor(out=mixs[i2][:], in0=tmp3[i2][:], in1=mod["B2"][:], op=OP.add), r=[tk], w=[mk])
            S.op("pool", lambda e: e.tensor_copy(out=hx2bt[i2][:], in_=mixs[i2][:]), r=[mk], w=["hx2bt%d" % i2])
            S.dma("sp", "h2st", h2_d[cs, :], hx2bt[i2][:], r=["hx2bt%d" % i2], w=[("h2d", ti)])
            for kc in range(8):
                S.op("pe", lambda e, kc=kc: e.transpose(out=tp_f[:, kc * 128:(kc + 1) * 128], in_=mixs[i2][:, kc * 128:(kc + 1) * 128], identity=ident_f[:]),
                     r=[mk], w=["tp_f"])
            S.op("act", lambda e: e.activation(out=hx2T[i2][:].rearrange("p a b -> p (a b)"), in_=tp_f[:], func=AF.Copy), r=["tp_f"], w=["hx2T%d" % i2])
            for kc in range(8):
                S.op("pe", lambda e, kc=kc: e.matmul(ps_l[:, 0:NE], lhsT=hx2T[i2][:, kc, :], rhs=wr[:, kc, :], start=(kc == 0), stop=(kc == 7)),
                     r=["hx2T%d" % i2, "wr"], w=["ps_l"])
            S.op("dve", lambda e: e.tensor_tensor(out=lg[:, ti, :], in0=ps_l[:, 0:NE], in1=brt[:], op=OP.add), r=["ps_l", "brt"], w=[("lg", ti)])
        S.barrier()
        add_dbg("lg", lg[:], [128, NTO, NE], F32)
        flush_dbg()
    sc2.close()
    scA.close()

    with ExitStack() as pr_:
        def sbr(name, shape, dt):
            return pr_.enter_context(nc.sbuf_tensor("r_" + name, list(shape), dt))
        t8 = sbr("t8", [128, NTO, 8], F32)
        mask = sbr("mask", [128, NTO, NE], F32)
        negmax = sbr("negmax", [128, NTO], F32)
        ex = sbr("ex", [128, NTO, NE], F32)
        den = sbr("den", [128, NTO], F32)
        gate = sbr("gate", [128, NTO, NE], F32)
        keyt = sbr("keyt", [128, NE], F32)
        k8 = sbr("k8", [128, NTO, 8], F32)
        maskb = sbr("maskb", [128, NTO, NE], BF16)
        iota1 = sbr("iota1", [128, NE + NSLOT + 1], F32)
        tri_f = sbr("tri_f", [128, 128], F32)
        tri_b = sbr("tri_b", [128, 128], BF16)
        uu = sbr("uu", [NE, 2 * NE], F32)
        nbf = sbr("nbf", [NE, 128], F32)
        nbi = sbr("nbi", [NE, 128], I32)
        blk = sbr("blk", [128, 2 * NE], F32)
        dest = sbr("dest", [128, NTO, NE], F32)
        oh = [sbr("oh%d" % i, [128, NE], F32) for i in range(2)]
        pr1 = [sbr("pr1%d" % i, [128, NE], F32) for i in range(2)]
        pr2 = [sbr("pr2%d" % i, [128, NE], F32) for i in range(2)]
        d4f = sbr("d4f", [128, NTO * 4], F32)
        g4 = sbr("g4", [128, NTO * 4], F32)
        cmp_ = sbr("cmp", [128, NSLOT, NE], F32)
        bef = sbr("bef", [128, 2 * NSLOT], F32)
        idxf = sbr("idxf", [128, NSLOT], F32)
        p_t = pr_.enter_context(nc.psum_tensor("p_t", [128, 512], F32))
        p_b = pr_.enter_context(nc.psum_tensor("p_b", [128, 512], F32))
        p_r = pr_.enter_context(nc.psum_tensor("p_r", [128, 512], F32))
        S.dma("sp", "c0", iota1[:, 0:NE], iota_d[:, 0:NE], w=["iota1"])
        S.dma("sp", "c0", iota1[:, NE:NE + NSLOT + 1], iota_d[:, NE:NE + NSLOT + 1], w=["iota1"])
        S.dma("sp", "c0", tri_f[:], tri_d, w=["tri_f"])
        S.dma("sp", "c0", uu[:], uu_d, w=["uu"])
        S.op("dve", lambda e: e.tensor_copy(out=tri_b[:], in_=tri_f[:]), r=["tri_f"], w=["tri_b"])
        for ti in range(NTO):
            S.op("dve", lambda e: e.max(out=t8[:, ti, :], in_=lg[:, ti, :]), r=[], w=["t8"])
        S.op("dve", lambda e: e.tensor_scalar(out=negmax[:], in0=t8[:, :, 0], scalar1=-1.0, scalar2=None, op0=OP.mult), r=["t8"], w=["negmax"])
        for ti in range(NTO):
            S.op("dve", lambda e: e.tensor_scalar(out=mask[:, ti, :], in0=lg[:, ti, :], scalar1=t8[:, ti, 3:4], scalar2=None, op0=OP.is_ge),
                 r=["t8"], w=["mask"])
            S.op("act", lambda e: e.activation(out=ex[:, ti, :], in_=lg[:, ti, :], func=AF.Exp, bias=negmax[:, ti:ti + 1], scale=1.0),
                 r=["negmax"], w=["ex"])
        S.op("dve", lambda e: e.tensor_tensor(out=ex[:], in0=ex[:], in1=mask[:], op=OP.mult), r=["ex", "mask"], w=["ex"])
        S.op("dve", lambda e: e.reduce_sum(out=den[:], in_=ex[:], axis=AX.X), r=["ex"], w=["den"])
        S.op("dve", lambda e: e.reciprocal(out=den[:], in_=den[:]), r=["den"], w=["den"])
        for ti in range(NTO):
            S.op("dve", lambda e: e.tensor_scalar(out=gate[:, ti, :], in0=ex[:, ti, :], scalar1=den[:, ti:ti + 1], scalar2=None, op0=OP.mult),
                 r=["ex", "den"], w=["gate"])
            S.op("dve", lambda e: e.tensor_tensor(out=keyt[:], in0=mask[:, ti, :], in1=iota1[:, 0:NE], op=OP.mult), r=["mask", "iota1"], w=["keyt"])
            S.op("dve", lambda e: e.max(out=k8[:, ti, :], in_=keyt[:]), r=["keyt"], w=["k8"])
        S.op("dve", lambda e: e.tensor_copy(out=maskb[:], in_=mask[:]), r=["mask"], w=["maskb"])
        for ti in range(NTO):
            S.op("pe", lambda e: e.matmul(p_t[0:NE, 0:128], lhsT=maskb[:, ti, :], rhs=ones_b[:], start=(ti == 0), stop=(ti == NTO - 1)),
                 r=["maskb", "ones_b"], w=["p_t"])
        S.op("dve", lambda e: e.tensor_scalar(out=nbf[:], in0=p_t[0:NE, 0:128], scalar1=127.0, scalar2=None, op0=OP.add), r=["p_t"], w=["nbf"])
        S.op("dve", lambda e: e.tensor_copy(out=nbi[:], in_=nbf[:]), r=["nbf"], w=["nbi"])
        S.op("dve", lambda e: e.tensor_single_scalar(out=nbi[:], in_=nbi[:], scalar=7, op=OP.arith_shift_right), r=["nbi"], w=["nbi"])
        S.op("dve", lambda e: e.tensor_copy(out=nbf[:], in_=nbi[:]), r=["nbi"], w=["nbf"])
        S.op("pe", lambda e: e.matmul(p_b[:, 0:2 * NE], lhsT=nbf[:], rhs=uu[:], start=True, stop=True), r=["nbf", "uu"], w=["p_b"])
        S.op("dve", lambda e: e.tensor_copy(out=blk[:], in_=p_b[:, 0:2 * NE]), r=["p_b"], w=["blk"])
        for ti in range(NTO):
            for tp_ in range(ti):
                S.op("pe", lambda e, tp_=tp_: e.matmul(p_r[:, ti * NE:(ti + 1) * NE], lhsT=ones_b[:], rhs=maskb[:, tp_, :], start=(tp_ == 0), stop=False),
                     r=["maskb"], w=["p_r"])
            S.op("pe", lambda e: e.matmul(p_r[:, ti * NE:(ti + 1) * NE], lhsT=tri_b[:], rhs=maskb[:, ti, :], start=(ti == 0), stop=True),
                 r=["maskb", "tri_b"], w=["p_r"])
        for ti in range(NTO):
            S.op("dve", lambda e: e.scalar_tensor_tensor(out=dest[:, ti, :], in0=blk[:, 0:NE], scalar=128.0, in1=p_r[:, ti * NE:(ti + 1) * NE],
                                                         op0=OP.mult, op1=OP.add), r=["blk", "p_r"], w=["dest"])
        oh3 = sbr("oh3", [128, NTO, NE], F32)
        pr3 = sbr("pr3", [128, NTO, NE], F32)
        d4v = d4f[:].rearrange("p (t k) -> p t k", k=4)
        g4v = g4[:].rearrange("p (t k) -> p t k", k=4)
        for k in range(4):
            S.op("dve", lambda e: e.tensor_tensor(out=oh3[:], in0=iota1[:, 0:NE].unsqueeze(1).to_broadcast([128, NTO, NE]),
                                                  in1=k8[:, :, k:k + 1].to_broadcast([128, NTO, NE]), op=OP.is_equal), r=["k8", "iota1"], w=["oh3"])
            S.op("dve", lambda e: e.tensor_tensor(out=pr3[:], in0=oh3[:], in1=dest[:], op=OP.mult), r=["oh3", "dest"], w=["pr3"])
            S.op("dve", lambda e: e.reduce_sum(out=d4v[:, :, k], in_=pr3[:], axis=AX.X), r=["pr3"], w=["d4f"])
            S.op("dve", lambda e: e.tensor_tensor(out=pr3[:], in0=oh3[:], in1=gate[:], op=OP.mult), r=["oh3", "gate"], w=["pr3"])
            S.op("dve", lambda e: e.reduce_sum(out=g4v[:, :, k], in_=pr3[:], axis=AX.X), r=["pr3"], w=["g4"])
        S.op("dve", lambda e: e.tensor_copy(out=d4i[:], in_=d4f[:]), r=["d4f"], w=["d4i"])
        S.op("dve", lambda e: e.tensor_tensor(out=cmp_[:], in0=blk[:, NE:2 * NE].unsqueeze(1).to_broadcast([128, NSLOT, NE]),
                                              in1=iota1[:, NE:NE + NSLOT].unsqueeze(2).to_broadcast([128, NSLOT, NE]), op=OP.is_le),
             r=["blk", "iota1"], w=["cmp"])
        S.op("dve", lambda e: e.reduce_sum(out=bef[:, 0:NSLOT], in_=cmp_[:], axis=AX.X), r=["cmp"], w=["bef"])
        S.op("dve", lambda e: e.tensor_scalar(out=bef[:, 0:NSLOT], in0=bef[:, 0:NSLOT], scalar1=float(NE - 1), scalar2=None, op0=OP.min), r=["bef"], w=["bef"])
        S.op("dve", lambda e: e.memset(bef[:, NSLOT:NSLOT + 2], 1.0), r=[], w=["bef"])
        S.op("dve", lambda e: e.tensor_tensor(out=bef[:, NSLOT + 2:2 * NSLOT], in0=bef[:, 2:NSLOT], in1=bef[:, 0:NSLOT - 2], op=OP.not_equal), r=["bef"], w=["bef"])
        S.op("dve", lambda e: e.tensor_copy(out=beci[:], in_=bef[:]), r=["bef"], w=["beci"])
        BIG = 1000000.0
        S.op("dve", lambda e: e.scalar_tensor_tensor(out=idxf[:], in0=bef[:, 0:NSLOT], scalar=128.0,
                                                     in1=iota1[:, NE + NSLOT:NE + NSLOT + 1].to_broadcast([128, NSLOT]), op0=OP.mult, op1=OP.add),
             r=["bef", "iota1"], w=["idxf"])
        S.op("dve", lambda e: e.tensor_scalar(out=idxf[:], in0=idxf[:], scalar1=-BIG, scalar2=None, op0=OP.add), r=["idxf"], w=["idxf"])
        S.op("dve", lambda e: e.tensor_tensor(out=idxf[:], in0=idxf[:], in1=bef[:, NSLOT:2 * NSLOT], op=OP.mult), r=["idxf", "bef"], w=["idxf"])
        S.op("dve", lambda e: e.tensor_scalar(out=idxf[:], in0=idxf[:], scalar1=BIG, scalar2=None, op0=OP.add), r=["idxf"], w=["idxf"])
        S.op("dve", lambda e: e.tensor_copy(out=idxw[:], in_=idxf[:]), r=["idxf"], w=["idxw"])
        S.barrier()
        add_dbg("d4i", d4i[:], [128, NTO * 4], I32)
        add_dbg("g4", g4[:], [128, NTO * 4], F32)
        add_dbg("beci", beci[0:1, :], [1, 2 * NSLOT], I32)
        add_dbg("idxw", idxw[:], [128, NSLOT], I32)
        flush_dbg()
        hx2r = [[sbr("hx2r%d_%d" % (i, k), [128, D + 32], BF16) for k in range(4)] for i in range(2)]
        ghl = sbr("ghl", [128, NTO * 4, 2], BF16)
        gres = sbr("gres", [128, NTO * 4], F32)
        S.op("dve", lambda e: e.tensor_copy(out=ghl[:, :, 0], in_=g4[:]), r=["g4"], w=["ghl"])
        S.op("dve", lambda e: e.tensor_tensor(out=gres[:], in0=g4[:], in1=ghl[:, :, 0], op=OP.subtract), r=["g4", "ghl"], w=["gres"])
        S.op("dve", lambda e: e.tensor_copy(out=ghl[:, :, 1], in_=gres[:]), r=["gres"], w=["ghl"])
        for i_ in range(2):
            for k_ in range(4):
                S.op("dve", lambda e: e.memset(hx2r[i_][k_][:, D:D + 32], 0.0), w=["hx2r%d_%d" % (i_, k_)])
        for ti in range(NTO):
            i2 = ti % 2
            for k in range(4):
                col = ti * 4 + k
                hk = "hx2r%d_%d" % (i2, k)
                S.dma("sp", "h2ld" + hk, hx2r[i2][k][:, 0:D], h2_d[ti * 128:(ti + 1) * 128, :], w=[hk])
                S.op("dve", lambda e: e.tensor_copy(out=hx2r[i2][k][:, D:D + 2], in_=ghl[:, col, :]), r=["ghl"], w=[hk])
                S.idma("sc_x%d" % k, r=[hk], w=["xs_d"], out=xs_d, out_offset=bass.IndirectOffsetOnAxis(ap=d4i[:, col:col + 1], axis=0),
                       in_=hx2r[i2][k][:], in_offset=None)
        S.barrier()
    if stage < 8:
        close_all(); S.barrier(); es.close(); return nc, list(dbg_out)

    n_slots = NSLOT if stage >= 9 else 4
    with ExitStack() as pm_:
        def sbm(name, shape, dt):
            return pm_.enter_context(nc.sbuf_tensor("m_" + name, list(shape), dt))

        def psm(name, shape, dt):
            return pm_.enter_context(nc.psum_tensor("m_" + name, list(shape), dt))
        wg = [sbm("wg%d" % i, [128, 9, D], BF16) for i in range(2)]
        wu_ = [sbm("wu%d" % i, [128, 9, D], BF16) for i in range(2)]
        wd = [sbm("wd%d" % i, [128, 9, D], BF16) for i in range(2)]
        bg = [wg[i][:, 8, :] for i in range(2)]
        bu = [wu_[i][:, 8, :] for i in range(2)]
        bd = [wd[i][:, 8, :] for i in range(2)]
        xsb = [sbm("xsb%d" % i, [128, D + 32], BF16) for i in range(2)]
        xT = [sbm("xT%d" % i, [128, 8, 128], BF16) for i in range(2)]
        gm = sbm("gm", [128, D], F32)
        sg = sbm("sg", [128, D], F32)
        u1 = sbm("u1", [128, D], F32)
        actb = sbm("actb", [128, D], BF16)
        actT = sbm("actT", [128, 8, 128], BF16)
        ysb = [sbm("ysb%d" % i, [128, D], F32) for i in range(2)]
        gsc = [sbm("gsc%d" % i, [128, 1], F32) for i in range(2)]
        tpx = psm("tpx", [128, 8, 128], BF16)
        psg = psm("psg", [128, D], F32)
        psu = psm("psu", [128, D], F32)
        tpa = psm("tpa", [128, 8, 128], BF16)
        psy = psm("psy", [128, D], F32)
        rb_ = pm_.enter_context(nc.gpsimd.register("rb_"))
        nc.gpsimd.reg_mov(rb_, NE * 128 - 1)
        bv = nc.gpsimd.snap(rb_)

        def load_w(b):
            p = b % 2
            for (wt, wsrc, key) in ((wg[p], wb_gate, "wg%d" % p), (wu_[p], wb_up, "wu%d" % p), (wd[p], wb_down, "wd%d" % p)):
                S.idma("wld%d_%s" % (p, key), r=[], w=[key], out=wt[:].rearrange("p a n -> p (a n)"), out_offset=None, in_=wsrc,
                       in_offset=bass.IndirectOffsetOnAxis(ap=idxw[:, b:b + 1], axis=0), bounds_check=bv, oob_is_err=False)

        load_w(0)
        for b in range(n_slots):
            p = b % 2
            if b + 1 < n_slots:
                load_w(b + 1)
            rs = slice(b * 128, (b + 1) * 128)
            if b == 0:
                S.dma("sp", "xsl%d" % p, xsb[p][:], xs_d[rs, :], w=["xsb%d" % p])
            for kc in range(8):
                S.op("pe", lambda e, kc=kc: e.transpose(out=tpx[:, kc, :], in_=xsb[p][:, kc * 128:(kc + 1) * 128], identity=ident_b[:]),
                     r=["xsb%d" % p], w=["tpx"])
            S.op("act", lambda e: e.activation(out=xT[p][:], in_=tpx[:], func=AF.Copy), r=["tpx"], w=["xT%d" % p])
            for (pt, wt, bt, pk, wk) in ((psg, wg[p], bg[p], "psg", "wg%d" % p), (psu, wu_[p], bu[p], "psu", "wu%d" % p)):
                for hf in range(2):
                    hs = slice(hf * 512, (hf + 1) * 512)
                    for kc in range(8):
                        S.op("pe", lambda e, kc=kc, pt=pt, wt=wt, hs=hs: e.matmul(pt[:, hs], lhsT=xT[p][:, kc, :], rhs=wt[:, kc, hs], start=(kc == 0), stop=False),
                             r=["xT%d" % p, wk], w=[pk])
                    S.op("pe", lambda e, pt=pt, bt=bt, hs=hs: e.matmul(pt[:, hs], lhsT=ones_b[0:1, :], rhs=bt[0:1, hs], start=False, stop=True),
                         r=[wk], w=[pk])
            if b + 1 < n_slots:
                S.dma("sp", "xsl%d" % ((b + 1) % 2), xsb[(b + 1) % 2][:], xs_d[(b + 1) * 128:(b + 2) * 128, :], w=["xsb%d" % ((b + 1) % 2)])
            S.op("dve", lambda e: e.tensor_scalar(out=gm[:], in0=psg[:], scalar1=7.0, scalar2=None, op0=OP.min), r=["psg"], w=["gm"])
            S.op("act", lambda e: e.activation(out=sg[:], in_=gm[:], func=AF.Sigmoid, scale=1.702), r=["gm"], w=["sg"])
            S.op("dve", lambda e: e.tensor_scalar(out=u1[:], in0=psu[:], scalar1=7.0, scalar2=-7.0, op0=OP.min, op1=OP.max), r=["psu"], w=["u1"])
            S.op("dve", lambda e: e.scalar_tensor_tensor(out=u1[:], in0=u1[:], scalar=1.0, in1=gm[:], op0=OP.add, op1=OP.mult), r=["u1", "gm"], w=["u1"])
            S.op("dve", lambda e: e.tensor_tensor(out=actb[:], in0=u1[:], in1=sg[:], op=OP.mult), r=["u1", "sg"], w=["actb"])
            for kc in range(8):
                S.op("pe", lambda e, kc=kc: e.transpose(out=tpa[:, kc, :], in_=actb[:, kc * 128:(kc + 1) * 128], identity=ident_b[:]),
                     r=["actb"], w=["tpa"])
            S.op("act", lambda e: e.activation(out=actT[:], in_=tpa[:], func=AF.Copy), r=["tpa"], w=["actT"])
            for hf in range(2):
                hs = slice(hf * 512, (hf + 1) * 512)
                for kc in range(8):
                    S.op("pe", lambda e, kc=kc, hs=hs: e.matmul(psy[:, hs], lhsT=actT[:, kc, :], rhs=wd[p][:, kc, hs], start=(kc == 0), stop=False),
                         r=["actT", "wd%d" % p], w=["psy"])
                S.op("pe", lambda e, hs=hs: e.matmul(psy[:, hs], lhsT=ones_b[0:1, :], rhs=bd[p][0:1, hs], start=False, stop=True),
                     r=["wd%d" % p], w=["psy"])
            S.op("dve", lambda e: e.tensor_tensor(out=gsc[p][:], in0=xsb[p][:, D:D + 1], in1=xsb[p][:, D + 1:D + 2], op=OP.add), r=["xsb%d" % p], w=["gsc%d" % p])
            S.op("act", lambda e: e.activation(out=ysb[p][:], in_=psy[:], func=AF.Copy, scale=gsc[p][:, 0:1]), r=["psy", "gsc%d" % p], w=["ysb%d" % p])
            S.dma("sp", "yst%d" % p, ys_d[rs, :], ysb[p][:], r=["ysb%d" % p], w=["ys_d"])
        S.barrier()

    with ExitStack() as pc_:
        def sbc_(name, shape, dt):
            return pc_.enter_context(nc.sbuf_tensor("f_" + name, list(shape), dt))
        yg = [[sbc_("yg%d_%d" % (i, k), [128, D], F32) for k in range(4)] for i in range(2)]
        x1r = [sbc_("x1r%d" % i, [128, D], F32) for i in range(2)]
        st4 = [sbc_("st4%d" % i, [128, 8], F32) for i in range(2)]
        junk4 = sbc_("junk4", [128, D], BF16)
        for ti in range(NTO):
            i2 = ti % 2
            cs = slice(ti * 128, (ti + 1) * 128)
            def fetch(t_):
                j2 = t_ % 2
                S.dma("sp", "x1r%d" % j2, x1r[j2][:], x1_d[t_ * 128:(t_ + 1) * 128, :], w=["x1r%d" % j2])
                for k in range(4):
                    col = t_ * 4 + k
                    S.idma("yg%d_%d" % (j2, k), r=[], w=["yg%d_%d" % (j2, k)], out=yg[j2][k][:], out_offset=None, in_=ys_d,
                           in_offset=bass.IndirectOffsetOnAxis(ap=d4i[:, col:col + 1], axis=0))
            if ti == 0:
                fetch(0)
            if ti + 1 < NTO:
                fetch(ti + 1)
            S.op("dve", lambda e: e.tensor_tensor(out=yg[i2][0][:], in0=yg[i2][0][:], in1=yg[i2][1][:], op=OP.add),
                 r=["yg%d_0" % i2, "yg%d_1" % i2], w=["yg%d_0" % i2])
            S.op("dve", lambda e: e.tensor_tensor(out=yg[i2][2][:], in0=yg[i2][2][:], in1=yg[i2][3][:], op=OP.add),
                 r=["yg%d_2" % i2, "yg%d_3" % i2], w=["yg%d_2" % i2])
            S.op("dve", lambda e: e.tensor_tensor(out=yg[i2][0][:], in0=yg[i2][0][:], in1=yg[i2][2][:], op=OP.add),
                 r=["yg%d_0" % i2, "yg%d_2" % i2], w=["yg%d_0" % i2])
            S.op("act", lambda e: e.activation(out=junk4[:], in_=yg[i2][0][:], func=AF.Square, scale=1.0 / 32.0, accum_out=st4[i2][:, 0:1]),
                 r=["yg%d_0" % i2], w=["junk4", "st4%d" % i2])
            rstd(st4[i2], 0, 1, "st4%d" % i2)
            S.op("dve", lambda e: e.scalar_tensor_tensor(out=yg[i2][1][:], in0=yg[i2][0][:], scalar=st4[i2][:, 1:2], in1=mod["G2"][:], op0=OP.mult, op1=OP.mult),
                 r=["yg%d_0" % i2, "st4%d" % i2], w=["yg%d_1" % i2])
            S.op("dve", lambda e: e.tensor_tensor(out=yg[i2][1][:], in0=yg[i2][1][:], in1=x1r[i2][:], op=OP.add),
                 r=["yg%d_1" % i2, "x1r%d" % i2], w=["yg%d_1" % i2])
            S.dma("sp", "ost%d" % i2, out_d[cs, :], yg[i2][1][:], r=["yg%d_1" % i2], w=[("out", ti)])
        S.barrier()

    close_all()
    flush_dbg()
    S.barrier()
    es.close()
    return nc, list(dbg_out)


def _host_consts(j):
    half = 32
    inv = 1.0 / (10000.0 ** (np.arange(0, half, 2, dtype=np.float32) / half))

    def rope_tab(tok):
        row = (tok // 64).astype(np.float32)
        col = (tok % 64).astype(np.float32)
        ang = np.concatenate([row[:, None] * inv, col[:, None] * inv], axis=-1).astype(np.float32)
        cos, sin = np.cos(ang), np.sin(ang)
        cos2 = np.repeat(cos, 2, axis=1)
        sin2 = np.stack([-sin, sin], axis=-1).reshape(len(tok), 64)
        return np.ascontiguousarray(np.concatenate([cos2, sin2], axis=1).astype(np.float32))

    tok_all = np.arange(SEQ)
    tok_own = np.arange(j, SEQ, 4)
    lut_c = np.cos(2 * np.pi * np.arange(SEQ) / SEQ)
    lut_s = -np.sin(2 * np.pi * np.arange(SEQ) / SEQ)
    nk = (tok_all[:, None].astype(np.int64) * tok_own[None, :].astype(np.int64)) % SEQ
    dft_c = lut_c[nk].astype(ml_dtypes.bfloat16)
    dft_s = lut_s[nk].astype(ml_dtypes.bfloat16)
    cc = np.arange(64)
    ph = 2 * np.pi * np.outer(cc, cc) / 64.0
    sc = 1.0 / np.sqrt(SEQ * 64.0)
    Cc = np.cos(ph) * sc
    Sc = np.sin(ph) * sc
    cbd = np.zeros((256, 512), np.float32)
    for g in range(4):
        cbd[g * 64:(g + 1) * 64, g * 64:(g + 1) * 64] = Cc
        cbd[g * 64:(g + 1) * 64, 256 + g * 64:256 + (g + 1) * 64] = Sc
    p = np.arange(128)
    tri = (p[:, None] < p[None, :]).astype(np.float32)
    e = np.arange(NE)
    uu = np.concatenate([(e[:, None] < e[None, :]), (e[:, None] <= e[None, :])], axis=1).astype(np.float32)
    return {
        "rope_all": rope_tab(tok_all), "rope_own": rope_tab(tok_own),
        "dft_c": dft_c, "dft_s": dft_s, "cbd": cbd,
        "ident_d": np.eye(128, dtype=np.float32), "tri_d": tri,
        "iota_d": np.concatenate([np.tile(np.concatenate([np.arange(1, NE + 1), np.arange(NSLOT)]).astype(np.float32)[None, :], (128, 1)),
                                  np.arange(128, dtype=np.float32)[:, None]], axis=1),
        "uu_d": uu,
    }


_CONST_CACHE = {}


def _relayout(w, b):
    w = np.asarray(w, dtype=np.float32).reshape(NE, 8, 128, D).transpose(0, 2, 1, 3).reshape(NE, 128, 8 * D)
    bb = np.broadcast_to(np.asarray(b, dtype=np.float32).reshape(NE, 1, D), (NE, 128, D))
    return np.ascontiguousarray(np.concatenate([w, bb], axis=2).reshape(NE * 128, 9 * D))


def make_in_maps(inp):
    f = lambda a: np.ascontiguousarray(np.asarray(a, dtype=np.float32))
    x = f(inp["x"]); c = f(inp["c"]); ctx = f(inp["ctx"]); c_ctx = f(inp["c_ctx"])
    shared = {
        "cc_col": np.ascontiguousarray(c_ctx.reshape(8, 128).T),
        "w_mod": f(inp["w_mod"][0]), "b_mod": f(inp["b_mod"][0]).reshape(1, -1),
        "g_attn_pre": f(inp["norm_attn_pre"][0]).reshape(1, -1), "g_attn_post": f(inp["norm_attn_post"][0]).reshape(1, -1),
        "g_ffn_pre": f(inp["norm_ffn_pre"][0]).reshape(1, -1), "g_ffn_post": f(inp["norm_ffn_post"][0]).reshape(1, -1),
        "w_in": f(inp["w_in"][0]), "g_q": f(inp["norm_q_lat"][0]).reshape(1, -1), "g_kv": f(inp["norm_kv_lat"][0]).reshape(1, -1),
        "w_q_up": f(inp["w_q_up"][0]), "w_kv_up": f(inp["w_kv_up"][0]), "w_four": f(inp["w_fourier"][0]).reshape(256, 64),
        "w_out": f(inp["w_out"][0]), "w_router": f(inp["w_router"][0]), "b_router": f(inp["b_router"][0]).reshape(1, -1),
        "wa_gate": _relayout(inp["w_gate"][0], inp["b_gate"][0]),
        "wa_up": _relayout(inp["w_up"][0], inp["b_up"][0]),
        "wa_down": _relayout(inp["w_down"][0], inp["b_down"][0]),
    }
    maps = []
    for i in range(8):
        b, j = i // 4, i % 4
        if j not in _CONST_CACHE:
            _CONST_CACHE[j] = _host_consts(j)
        m = dict(shared)
        m.update(_CONST_CACHE[j])
        m["x_b"] = x[b]
        m["x_own"] = np.ascontiguousarray(x[b, j::4])
        m["ctx_b"] = ctx[b]
        m["c_col"] = np.ascontiguousarray(c[b].reshape(8, 128).T)
        maps.append(m)
    return maps


_NC_CACHE = {}


def kernel(**inputs):
    if "nc" not in _NC_CACHE:
        _NC_CACHE["nc"] = build_nc()[0]
    nc = _NC_CACHE["nc"]
    maps = make_in_maps(inputs)
    res = run_bass_kernel_spmd(nc, maps, core_ids=list(range(8)))
    out = np.empty((2, SEQ, D), np.float32)
    for i in range(8):
        b, j = i // 4, i % 4
        out[b, j::4] = res.results[i]["out"]
    return out
```
